# Optimizing a Trainium2 kernel written in Bass

```python
import math
import jax, jax.numpy as jnp
from jax import lax
import numpy as np

D_MODEL = 1024
BATCH = 4
SEQ = 4096
DEPTH = 4

HEAD_DIM = 64
ROPE_DIM = HEAD_DIM // 4
ROPE_THETA = 500000.0
NORM_EPS = 1e-6
Q_BLOCK = 128
SCALE = HEAD_DIM ** -0.5
NEG = -1e30
FORCE = 1e9

NSA_HEADS = 5
CMP_BLOCK = 32
CMP_STRIDE = 16
CMP_HIDDEN = 128
SLC_BLOCK = 64
SLC_TOPN = 16
WIN = 512

DSA_HEADS = 5
IDX_HEADS = 8
IDX_DIM = 32
IDX_ROPE_DIM = IDX_DIM // 4
DSA_TOPK_MAX = 256

DIL_PAIRS = ((128, 1), (512, 4), (2048, 16))
DIL_HEADS_PER_GROUP = 2
DIL_HEADS = len(DIL_PAIRS) * DIL_HEADS_PER_GROUP
DIL_KEYS = DIL_PAIRS[0][0] // DIL_PAIRS[0][1] + 1

D_MIX = (NSA_HEADS + DSA_HEADS + DIL_HEADS) * HEAD_DIM

D_FF = 2816
CONV_WIDTH = 3

IN_SPLITS = (
    NSA_HEADS * HEAD_DIM,
    HEAD_DIM, HEAD_DIM,
    HEAD_DIM, HEAD_DIM,
    HEAD_DIM, HEAD_DIM,
    3 * NSA_HEADS,
    DSA_HEADS * HEAD_DIM, HEAD_DIM, HEAD_DIM,
    IDX_HEADS * IDX_DIM, IDX_DIM, IDX_HEADS,
    DIL_HEADS * HEAD_DIM, DIL_HEADS * HEAD_DIM, DIL_HEADS * HEAD_DIM,
)
D_IN = sum(IN_SPLITS)
IN_OFFSETS = tuple(int(o) for o in np.cumsum(IN_SPLITS)[:-1])

kernel_name = 'hybrid_nsa_dsa_dilated_convffn'


def rmsnorm(x, g):
    xf = x.astype(jnp.float32)
    y = xf * lax.rsqrt(jnp.mean(xf * xf, axis=-1, keepdims=True) + NORM_EPS)
    return (y * g.astype(jnp.float32)).astype(x.dtype)


def rope_tables(L, rot_dim, dtype):
    inv = 1.0 / (ROPE_THETA ** (np.arange(0, rot_dim, 2, dtype=np.float32) / np.float32(rot_dim)))
    ang = np.arange(L, dtype=np.float32)[:, None] * inv[None, :]
    return jnp.asarray(np.cos(ang), dtype=dtype), jnp.asarray(np.sin(ang), dtype=dtype)


def apply_rope(x, cos, sin):
    r = 2 * cos.shape[-1]
    x1, x2, xp = x[..., : r // 2], x[..., r // 2 : r], x[..., r:]
    c, s = cos[None, :, None, :], sin[None, :, None, :]
    return jnp.concatenate([x1 * c - x2 * s, x2 * c + x1 * s, xp], axis=-1)


def rope1(k, cos, sin):
    return apply_rope(k[:, :, None], cos, sin)[:, :, 0]


def masked_softmax(s, mask):
    s = jnp.where(mask, s, NEG)
    m = jnp.max(s, axis=-1, keepdims=True)
    e = jnp.where(mask, jnp.exp(s - m), 0.0)
    den = jnp.maximum(jnp.sum(e, axis=-1, keepdims=True), 1e-30)
    return e / den, m + jnp.log(den)


def to_chunks(a):
    b, l = a.shape[:2]
    return jnp.moveaxis(a.reshape(b, l // Q_BLOCK, Q_BLOCK, *a.shape[2:]), 1, 0)


def from_chunks(a):
    a = jnp.moveaxis(a, 0, 1)
    return a.reshape(a.shape[0], a.shape[1] * a.shape[2], *a.shape[3:])


def q_positions(L):
    return jnp.arange(L).reshape(L // Q_BLOCK, Q_BLOCK)


def gather_rows(table, idx):
    return jax.vmap(lambda t, i: t[i])(table, idx)


def nsa_mixer(q, kc, vc, ks, vs, kw, vw, g, cmp_pos, cmp_w1, cmp_w2, cos, sin):
    B, L, H, D = q.shape
    t = jnp.arange(L)
    n_cmp = (L - CMP_BLOCK) // CMP_STRIDE + 1
    c_start = np.arange(n_cmp) * CMP_STRIDE
    blk_idx = c_start[:, None] + np.arange(CMP_BLOCK)[None, :]
    raw = jnp.stack([kc, vc], axis=0)[:, :, blk_idx] + cmp_pos[:, None, None]
    hid = jax.nn.gelu(jnp.einsum('cbnf,cfh->cbnh', raw.reshape(2, B, n_cmp, CMP_BLOCK * D), cmp_w1))
    kv_cmp = jnp.einsum('cbnh,chd->cbnd', hid, cmp_w2)
    k_cmp, v_cmp = kv_cmp[0], kv_cmp[1]
    s_c = jnp.einsum('bthd,bnd->bhtn', q, k_cmp).astype(jnp.float32) * SCALE
    mask_c = jnp.asarray(c_start + CMP_BLOCK - 1)[None, :] <= t[:, None]
    p_c, _ = masked_softmax(s_c, mask_c)
    o_cmp = jnp.einsum('bhtn,bnd->bthd', p_c.astype(v_cmp.dtype), v_cmp)
    n_slc = L // SLC_BLOCK
    s_start = np.arange(n_slc) * SLC_BLOCK
    overlap = ((c_start[:, None] < s_start[None, :] + SLC_BLOCK)
               & (c_start[:, None] + CMP_BLOCK > s_start[None, :])).astype(np.float32)
    imp = jnp.einsum('bhtn,nj->btj', p_c, jnp.asarray(overlap))
    cur = t // SLC_BLOCK
    j = jnp.arange(n_slc)
    valid_blk = j[None, :] <= cur[:, None]
    forced = (j[None, :] == 0) | (j[None, :] == cur[:, None]) | (j[None, :] == cur[:, None] - 1)
    imp = jnp.where(forced, FORCE, jnp.where(valid_blk, imp, NEG))
    n_top = min(SLC_TOPN, n_slc)
    top_s, sel = lax.top_k(imp, n_top)
    sel_ok = top_s > 0.5 * NEG
    q_r = apply_rope(q, cos, sin)
    ks_blocks = rope1(ks, cos, sin).reshape(B, n_slc, SLC_BLOCK, D)
    vs_blocks = vs.reshape(B, n_slc, SLC_BLOCK, D)
    in_blk = jnp.arange(SLC_BLOCK)

    def sel_chunk(args):
        qc, selc, okc, tc = args
        kg = gather_rows(ks_blocks, selc).reshape(B, Q_BLOCK, n_top * SLC_BLOCK, D)
        vg = gather_rows(vs_blocks, selc).reshape(B, Q_BLOCK, n_top * SLC_BLOCK, D)
        kpos = (selc[..., None] * SLC_BLOCK + in_blk).reshape(B, Q_BLOCK, -1)
        ok = jnp.broadcast_to(okc[..., None], okc.shape + (SLC_BLOCK,)).reshape(B, Q_BLOCK, -1)
        ok = ok & (kpos <= tc[None, :, None])
        s = jnp.einsum('bqhd,bqkd->bqhk', qc, kg).astype(jnp.float32) * SCALE
        p, _ = masked_softmax(s, ok[:, :, None, :])
        return jnp.einsum('bqhk,bqkd->bqhd', p.astype(vg.dtype), vg)

    o_slc = from_chunks(lax.map(sel_chunk, (to_chunks(q_r), to_chunks(sel), to_chunks(sel_ok), q_positions(L))))
    nb = L // Q_BLOCK
    kw_p = jnp.pad(rope1(kw, cos, sin), ((0, 0), (WIN, 0), (0, 0)))
    vw_p = jnp.pad(vw, ((0, 0), (WIN, 0), (0, 0)))
    span_idx = np.arange(nb)[:, None] * Q_BLOCK + np.arange(Q_BLOCK + WIN)[None, :]
    kband, vband = kw_p[:, span_idx], vw_p[:, span_idx]
    s_w = jnp.einsum('bnqhd,bnkd->bnhqk', q_r.reshape(B, nb, Q_BLOCK, H, D), kband).astype(jnp.float32) * SCALE
    key_pos = span_idx - WIN
    qpos = np.arange(nb)[:, None] * Q_BLOCK + np.arange(Q_BLOCK)[None, :]
    diff = qpos[:, :, None] - key_pos[:, None, :]
    mask_w = jnp.asarray((diff >= 0) & (diff < WIN) & (key_pos[:, None, :] >= 0))
    p_w, _ = masked_softmax(s_w, mask_w[None, :, None])
    o_win = jnp.einsum('bnhqk,bnkd->bnqhd', p_w.astype(vband.dtype), vband).reshape(B, L, H, D)
    gs = jax.nn.sigmoid(g)
    return gs[..., 0:1] * o_cmp + gs[..., 1:2] * o_slc + gs[..., 2:3] * o_win


def dsa_mixer(q, k, v, qi, ki, wi, cos_h, sin_h, cos_i, sin_i):
    B, L = q.shape[:2]
    q = apply_rope(q, cos_h, sin_h)
    k = rope1(k, cos_h, sin_h)
    qi = apply_rope(qi, cos_i, sin_i)
    ki = rope1(ki, cos_i, sin_i)
    top = min(DSA_TOPK_MAX, L // 4)
    keys = jnp.arange(L)

    def chunk(args):
        qc, qic, wc, tc = args
        dots = jnp.einsum('bqhi,bsi->bqhs', qic, ki).astype(jnp.float32) * IDX_DIM ** -0.5
        score = jnp.einsum('bqh,bqhs->bqs', wc.astype(jnp.float32) * IDX_HEADS ** -0.5, jax.nn.relu(dots))
        score = jnp.where(keys[None, None, :] <= tc[None, :, None], score, NEG)
        top_s, sel = lax.top_k(score, top)
        ok = top_s > 0.5 * NEG
        kg, vg = gather_rows(k, sel), gather_rows(v, sel)
        s = jnp.einsum('bqhd,bqkd->bqhk', qc, kg).astype(jnp.float32) * SCALE
        p, _ = masked_softmax(s, ok[:, :, None, :])
        return jnp.einsum('bqhk,bqkd->bqhd', p.astype(vg.dtype), vg)

    return from_chunks(lax.map(chunk, (to_chunks(q), to_chunks(qi), to_chunks(wi), q_positions(L))))


def dilated_mixer(q, k, v, cos, sin):
    B, L, _, D = q.shape
    G, Hg = len(DIL_PAIRS), DIL_HEADS_PER_GROUP
    q = apply_rope(q, cos, sin).reshape(B, L, G, Hg, D)
    k = apply_rope(k, cos, sin).reshape(B, L, G, Hg, D)
    v = v.reshape(B, L, G, Hg, D)
    offs = jnp.asarray(np.stack([np.arange(DIL_KEYS) * r for _, r in DIL_PAIRS]))
    gsel = jnp.arange(G)[None, :, None]

    def chunk(args):
        qc, tc = args
        kpos = tc[:, None, None] - offs[None]
        ok = kpos >= 0
        kidx = jnp.maximum(kpos, 0)
        kg, vg = k[:, kidx, gsel], v[:, kidx, gsel]
        s = jnp.einsum('bqghd,bqgkhd->bqghk', qc, kg).astype(jnp.float32) * SCALE
        p, lse = masked_softmax(s, ok[None, :, :, None, :])
        o = jnp.einsum('bqghk,bqgkhd->bqghd', p.astype(vg.dtype), vg)
        return o, lse[..., 0]

    o, lse = lax.map(chunk, (to_chunks(q), q_positions(L)))
    o, lse = from_chunks(o), from_chunks(lse)
    alpha = jax.nn.softmax(lse, axis=2)
    return (alpha[..., None].astype(o.dtype) * o).reshape(B, L, G * Hg, D)


def hybrid_mixer(h, w_in, cmp_pos, cmp_w1, cmp_w2, cos_h, sin_h, cos_i, sin_i):
    B, L, _ = h.shape
    (a_q, a_kc, a_vc, a_ks, a_vs, a_kw, a_vw, a_g,
     b_q, b_k, b_v, b_iq, b_ik, b_iw,
     c_q, c_k, c_v) = jnp.split(h @ w_in, IN_OFFSETS, axis=-1)
    heads = lambda a, n: a.reshape(B, L, n, -1)
    o_a = nsa_mixer(heads(a_q, NSA_HEADS), a_kc, a_vc, a_ks, a_vs, a_kw, a_vw, heads(a_g, NSA_HEADS),
                    cmp_pos, cmp_w1, cmp_w2, cos_h, sin_h)
    o_b = dsa_mixer(heads(b_q, DSA_HEADS), b_k, b_v, heads(b_iq, IDX_HEADS), b_ik, b_iw,
                    cos_h, sin_h, cos_i, sin_i)
    o_c = dilated_mixer(heads(c_q, DIL_HEADS), heads(c_k, DIL_HEADS), heads(c_v, DIL_HEADS), cos_h, sin_h)
    return jnp.concatenate([o_a.reshape(B, L, -1), o_b.reshape(B, L, -1), o_c.reshape(B, L, -1)], axis=-1)


def conv_glu_ffn(h, w_up, conv_w, conv_b, w_down):
    a, u = jnp.split(h @ w_up, 2, axis=-1)
    a = lax.conv_general_dilated(a, conv_w[:, None, :], window_strides=(1,),
                                 padding=((CONV_WIDTH - 1, 0),),
                                 dimension_numbers=('NWC', 'WIO', 'NWC'),
                                 feature_group_count=D_FF) + conv_b
    return (jax.nn.silu(a) * u) @ w_down


def setup_inputs(seed: int = 0) -> dict:
    key = jax.random.key(seed)
    ks = jax.random.split(key, 13)
    nrm = lambda k, shape, scale: jax.random.normal(k, shape, jnp.float32) * scale
    return {
        'x': nrm(ks[0], (BATCH, SEQ, D_MODEL), 1.0),
        'norm1_g': 1.0 + nrm(ks[1], (DEPTH, D_MODEL), 0.01),
        'w_in': nrm(ks[2], (DEPTH, D_MODEL, D_IN), D_MODEL ** -0.5),
        'cmp_pos': nrm(ks[3], (DEPTH, 2, CMP_BLOCK, HEAD_DIM), 0.02),
        'cmp_w1': nrm(ks[4], (DEPTH, 2, CMP_BLOCK * HEAD_DIM, CMP_HIDDEN), (CMP_BLOCK * HEAD_DIM) ** -0.5),
        'cmp_w2': nrm(ks[5], (DEPTH, 2, CMP_HIDDEN, HEAD_DIM), CMP_HIDDEN ** -0.5),
        'w_out': nrm(ks[6], (DEPTH, D_MIX, D_MODEL), D_MIX ** -0.5),
        'norm2_g': 1.0 + nrm(ks[7], (DEPTH, D_MODEL), 0.01),
        'w_up': nrm(ks[8], (DEPTH, D_MODEL, 2 * D_FF), D_MODEL ** -0.5),
        'conv_w': nrm(ks[9], (DEPTH, CONV_WIDTH, D_FF), CONV_WIDTH ** -0.5),
        'conv_b': nrm(ks[10], (DEPTH, D_FF), 0.01),
        'w_down': nrm(ks[11], (DEPTH, D_FF, D_MODEL), D_FF ** -0.5),
        'final_g': 1.0 + nrm(ks[12], (D_MODEL,), 0.01),
    }


def reference(x, norm1_g, w_in, cmp_pos, cmp_w1, cmp_w2, w_out, norm2_g, w_up, conv_w, conv_b, w_down, final_g):
    L = x.shape[1]
    cos_h, sin_h = rope_tables(L, ROPE_DIM, x.dtype)
    cos_i, sin_i = rope_tables(L, IDX_ROPE_DIM, x.dtype)
    for i in range(DEPTH):
        h = rmsnorm(x, norm1_g[i])
        mix = hybrid_mixer(h, w_in[i], cmp_pos[i], cmp_w1[i], cmp_w2[i], cos_h, sin_h, cos_i, sin_i)
        x = x + mix @ w_out[i]
        h = rmsnorm(x, norm2_g[i])
        x = x + conv_glu_ffn(h, w_up[i], conv_w[i], conv_b[i], w_down[i])
    return rmsnorm(x, final_g)
```

```python
import math
from contextlib import ExitStack
import numpy as np
import concourse.bass as bass
import concourse.mybir as mybir
from concourse.bass_utils import run_bass_kernel_spmd

F32 = mybir.dt.float32
BF16 = mybir.dt.bfloat16
ALU = mybir.AluOpType
AF = mybir.ActivationFunctionType
AX = mybir.AxisListType

L = 4096
NT = L // 128
DM = 1024
DIN = 2615
DFF = 2816
NFC = DFF // 128
EPS = 1e-6
MASKV = -30000.0
NEGBIG = -1e30
SCALE = 0.125
DIL = ((128, 1), (512, 4), (2048, 16))
DIL_O = (1, 4, 16)
DIL_R = (2, 5, 17)
O_AQ, O_AKC, O_AVC, O_AKS, O_AVS, O_AKW, O_AVW, O_AG = 0, 320, 384, 448, 512, 576, 640, 704
O_BQ, O_BK, O_BV, O_BIQ, O_BIK, O_BIW = 719, 1039, 1103, 1167, 1423, 1455
O_CQ, O_CK, O_CV = 1463, 1847, 2231
BIS_ITERS = 22
BIS_LO = -32.0

ENGS = ("pe", "act", "dve", "pool", "sp")
POOL_TO_DVE = False


class Prog:
    def __init__(self, nc, n_dma_slots=4):
        self.nc = nc
        self.streams = {e: [] for e in ENGS}
        self.cnt = {e: 0 for e in ENGS}
        self.sems = {}
        self.waited = {e: {} for e in ENGS}
        self.last_w = {}
        self.readers = {}
        self.n_dma_slots = n_dma_slots
        self.dma_rr = {e: 0 for e in ENGS}
        self.nops = 0
        self.enabled = True
        self.limit = None

    def stage(self, k):
        if self.limit is not None and k > self.limit:
            self.enabled = False

    def _deps(self, eng, reads, writes):
        deps = {}

        def add(src, val):
            if src == "pe" and eng == "pe":
                return
            if val > deps.get(src, 0):
                deps[src] = val
        for k in reads:
            lw = self.last_w.get(k)
            if lw:
                add(*lw)
        for k in writes:
            lw = self.last_w.get(k)
            if lw:
                add(*lw)
            for r in self.readers.get(k, ()):
                add(*r)
        return deps

    def _emit_waits(self, eng, deps):
        waits = []
        for src, val in deps.items():
            if val > self.waited[eng].get(src, 0):
                self.waited[eng][src] = val
                waits.append((src, val))
        return waits

    def _commit(self, src, val, reads, writes):
        for k in reads:
            lst = self.readers.setdefault(k, [])
            lst[:] = [r for r in lst if r[0] != src]
            lst.append((src, val))
        for k in writes:
            self.last_w[k] = (src, val)
            self.readers[k] = []

    def op(self, eng, fn, reads=(), writes=()):
        if not self.enabled:
            return
        if eng == "pool!":
            eng = "pool"
        elif eng == "pool" and POOL_TO_DVE:
            eng = "dve"
        psr = [k for k in reads if k.startswith("ps")]
        if psr:
            writes = list(writes) + psr
            reads = [k for k in reads if not k.startswith("ps")]
        deps = self._deps(eng, reads, writes)
        waits = self._emit_waits(eng, deps)
        self.cnt[eng] += 1
        self.streams[eng].append((waits, fn, (eng, 1)))
        self._commit(eng, self.cnt[eng], reads, writes)
        self.nops += 1

    def dma(self, eng, fn, reads=(), writes=()):
        if not self.enabled:
            return
        slot = self.dma_rr[eng]
        self.dma_rr[eng] = (slot + 1) % self.n_dma_slots
        src = ("dma", eng, slot)
        if src not in self.cnt:
            self.cnt[src] = 0
        deps = self._deps(eng, reads, writes)
        if self.cnt[src] > 0:
            deps[src] = max(deps.get(src, 0), self.cnt[src])
        waits = self._emit_waits(eng, deps)
        self.cnt[src] += 16
        self.streams[eng].append((waits, fn, (src, 16)))
        self._commit(src, self.cnt[src], reads, writes)
        self.nops += 1

    def barrier(self):
        snap = dict(self.cnt)
        for e in ENGS:
            deps = {s: v for s, v in snap.items() if v > 0 and not (s == "pe" and e == "pe")}
            waits = self._emit_waits(e, deps)
            if waits:
                self.streams[e].append((waits, None, None))

    def final_wait(self, eng, keys):
        deps = {}
        for k in keys:
            lw = self.last_w.get(k)
            if lw and lw[1] > deps.get(lw[0], 0):
                deps[lw[0]] = lw[1]
        waits = self._emit_waits(eng, deps)
        self.streams[eng].append((waits, None, None))

    def emit(self, stack):
        nc = self.nc
        for src in self.cnt:
            name = "s_" + (src if isinstance(src, str) else "_".join(str(x) for x in src))
            self.sems[src] = stack.enter_context(nc.semaphore(name))
        block = stack.enter_context(nc.Block())
        reg = {"pe": block.tensor, "act": block.scalar, "dve": block.vector, "pool": block.gpsimd, "sp": block.sync}
        for e in ENGS:
            stream = self.streams[e]
            if not stream:
                continue

            def body(engine, stream=stream):
                for waits, fn, inc in stream:
                    for src, val in waits:
                        engine.wait_ge(self.sems[src], val)
                    if fn is not None:
                        ins = fn(engine)
                        ins.then_inc(self.sems[inc[0]], inc[1])
            reg[e](body)


def _consts():
    t = np.arange(128)[:, None]
    s = np.arange(128)[None, :]
    tiles = []
    tiles.append(np.where(s <= t, 0.0, MASKV))
    tiles.append(np.where(s > t, 0.0, MASKV))
    for (w, r), og in zip(DIL, DIL_O):
        for o in range(og + 1):
            d = 128 * o + t - s
            ok = (d >= 0) & (d % r == 0) & (d <= 128 * r)
            tiles.append(np.where(ok, 0.0, MASKV))
    tiles.append(np.where(s <= t, 0.0, NEGBIG))
    bias = np.stack(tiles, axis=1).astype(np.float32)
    ident = np.eye(128, dtype=np.float32)
    i4 = np.concatenate([ident] * 4, axis=1)
    def tab(rot):
        inv = 1.0 / (500000.0 ** (np.arange(0, rot, 2, dtype=np.float32) / np.float32(rot)))
        ang = np.arange(L, dtype=np.float32)[:, None] * inv[None, :]
        cs = np.concatenate([np.cos(ang), np.sin(ang)], axis=1).astype(np.float32)
        return np.ascontiguousarray(cs.reshape(NT, 128, rot).transpose(1, 0, 2))
    cs_h = tab(16)
    cs_i = tab(8)
    n = np.arange(256)[:, None]
    j = np.arange(64)[None, :]
    ov = ((n >= 4 * j - 1) & (n <= 4 * j + 3) & (n <= 254)).astype(np.float32)
    ov = np.ascontiguousarray(ov.reshape(2, 128, 64).transpose(1, 0, 2))
    cm = []
    for (n_, nt_) in _cmp_pairs():
        base = 128 * n_ - 2048 * nt_ - 31
        cm.append(np.where(base + t - 16 * s >= 0, 0.0, MASKV))
    cmpb = np.stack(cm, axis=0).astype(np.float32)
    return dict(c_bias=bias, c_ident=ident, c_i4=i4, c_csh=cs_h, c_csi=cs_i, c_ov=ov, c_cmpb=cmpb)


def _cmp_pairs():
    out = []
    for n_ in range(NT):
        for nt_ in range(2):
            base = 128 * n_ - 2048 * nt_ - 31
            if base + 127 < 0:
                continue
            if base < 16 * 127:
                out.append((n_, nt_))
    return out


NB_BIAS = 2 + sum(o + 1 for o in DIL_O) + 1


def _dil_bias_idx(g, o):
    return 2 + sum(DIL_O[k] + 1 for k in range(g)) + o


def build(n_layers, final, dbg=False, nt_limit=None, stage_limit=None):
    nc = bass.Bass("TRN2", target_bir_lowering=False)
    NTT = NT if nt_limit is None else nt_limit

    def din(name, shape):
        return nc.dram_tensor(name, list(shape), F32, kind="ExternalInput").ap()

    x_in = din("x", [L, DM])
    w_norm1 = din("norm1_g", [n_layers, DM])
    w_in = din("w_in", [n_layers, DM, DIN])
    w_pos = din("cmp_pos", [n_layers, 2, 32, 64])
    w_c1 = din("cmp_w1", [n_layers, 2, 2048, 128])
    w_c2 = din("cmp_w2", [n_layers, 2, 128, 64])
    w_outd = din("w_out", [n_layers, DM, DM])
    w_norm2 = din("norm2_g", [n_layers, DM])
    w_up = din("w_up", [n_layers, DM, 2 * DFF])
    w_cw = din("conv_w", [n_layers, 3, DFF])
    w_cb = din("conv_b", [n_layers, DFF])
    w_down = din("w_down", [n_layers, DFF, DM])
    w_fin = din("final_g", [DM])
    c_bias = din("c_bias", [128, NB_BIAS, 128])
    c_ident = din("c_ident", [128, 128])
    c_i4 = din("c_i4", [128, 512])
    c_csh = din("c_csh", [128, NT, 16])
    c_csi = din("c_csi", [128, NT, 8])
    c_ov = din("c_ov", [128, 2, 64])
    cmp_pairs = _cmp_pairs()
    c_cmpb = din("c_cmpb", [len(cmp_pairs), 128, 128])
    y_out = nc.dram_tensor("y", [L, DM], F32, kind="ExternalOutput").ap()
    xs1 = nc.dram_tensor("xs1", [L, DM], F32, kind="Internal").ap()
    xs2 = nc.dram_tensor("xs2", [L, DM], F32, kind="Internal").ap()
    dbg_out = {}
    if dbg:
        dbg_out["d_pf"] = nc.dram_tensor("d_pf", [L, DIN], F32, kind="ExternalOutput").ap()
        dbg_out["d_mix"] = nc.dram_tensor("d_mix", [L, DM], F32, kind="ExternalOutput").ap()
        dbg_out["d_x1"] = nc.dram_tensor("d_x1", [L, DM], F32, kind="ExternalOutput").ap()

    st = ExitStack()
    with st:
        def SB(name, shape, dt):
            return st.enter_context(nc.sbuf_tensor(name, list(shape), dt))

        def PS(name, shape, dt):
            return st.enter_context(nc.psum_tensor(name, list(shape), dt))

        P = Prog(nc)
        P.limit = stage_limit

        psA = PS("psA", [128, 1024], F32)
        psB = PS("psB", [128, 1024], F32)
        psO = PS("psO", [128, 1024], F32)
        psP = PS("psP", [128, 512], F32)
        psT = PS("psT", [128, 1024], BF16)

        ident = SB("ident", [128, 128], BF16)
        i4 = SB("i4", [128, 512], BF16)
        biasT = SB("biasT", [128, NB_BIAS, 128], BF16)
        csh = SB("csh", [128, NT, 16], F32)
        csi = SB("csi", [128, NT, 8], F32)
        neghalf = SB("neghalf", [128, 1], F32)
        zerosb = SB("zerosb", [128, 128], BF16)

        UCOLS = 75520
        U = SB("U", [128, UCOLS], BF16)
        U2COLS = 7800
        U2 = SB("U2", [128, U2COLS], F32)
        ucur = [0]
        u2cur = [0]

        def carve(ncols):
            a = ucur[0]
            ucur[0] += ncols
            assert ucur[0] <= UCOLS, ucur[0]
            return U[:, a:a + ncols]

        def carve2(ncols):
            a = u2cur[0]
            u2cur[0] += ncols
            assert u2cur[0] <= U2COLS, u2cur[0]
            return U2[:, a:a + ncols]

        w_in_sb = carve(8 * DIN).rearrange("p (c n) -> p c n", c=8)
        w_out_sb = carve(8 * DM).rearrange("p (c n) -> p c n", c=8)
        KT_a = carve(L)
        KT_b = carve(L)
        kiT4 = carve(L)
        kcvT = carve(L)
        KT_c = [carve(DIL_R[g] * 128).rearrange("p (r t) -> p r t", t=128) for g in range(3)]
        Vs_a = carve(NT * 65).rearrange("p (n w) -> p n w", w=65)
        Vw_a = carve(5 * 65).rearrange("p (n w) -> p n w", w=65)
        V_b = carve(NT * 65).rearrange("p (n w) -> p n w", w=65)
        V_c = [carve(DIL_R[g] * 130).rearrange("p (r h w) -> p r h w", h=2, w=65) for g in range(3)]
        mb_d = carve(L)
        mb_s = mb_d
        Rbuf = carve(4 * 512).rearrange("p (h w) -> p h w", h=4)
        W1_sb = carve(32 * 128).rearrange("p (j h) -> p j h", j=32)
        TB_AQ, TB_AQR, TB_KA, TB_KCV, TB_B, TB_QI, TB_KI, TB_C = 0, 320, 960, 1088, 1216, 1600, 1856, 1984
        TBW = 1984 + 768
        Tb = carve(TBW)
        Dh = carve(8 * 128).rearrange("p (h t) -> p h t", h=8)
        QT_araw = carve(640)
        QT_ar = carve(640)
        QT_b = carve(640)
        qiT = carve(256).rearrange("p (a t) -> p a t", a=2)
        QT_c = carve(384).rearrange("p (a t) -> p a t", a=3)
        PT = [carve(640), carve(640)]
        mixb = carve(DM)
        att_end = ucur[0]
        ucur[0] = 0
        TF = 256
        w_up_sb = carve(8 * 2 * DFF).rearrange("p (c n) -> p c n", c=8)
        w_dn_sb = carve(NFC * DM).rearrange("p (c n) -> p c n", c=NFC)
        h2T = carve(8 * TF).rearrange("p (c t) -> p c t", c=8)
        gT = carve(NFC * TF).rearrange("p (c t) -> p c t", c=NFC)
        silb = carve(TF)
        ffn_end = ucur[0]
        score = carve2(L)
        Pf = carve2(DIN)
        xt1 = carve2(DM)
        xt = [xt1, xt1]
        x1t = xt1
        Ocmp = score[:, 0:645].rearrange("p (h w) -> p h w", h=5)
        Oslc = score[:, 645:970].rearrange("p (h w) -> p h w", h=5)
        Owin = score[:, 970:1295].rearrange("p (h w) -> p h w", h=5)
        Odsa = score[:, 1295:1620].rearrange("p (h w) -> p h w", h=5)
        Odil = score[:, 1620:2010].rearrange("p (g h w) -> p g h w", g=3, h=2)
        tmpo = score[:, 2010:2650].rearrange("p (a h d) -> p a h d", a=2, h=5)
        att2_end = u2cur[0]
        u2cur[0] = 0
        xf = carve2(2 * DM).rearrange("p (a n) -> p a n", a=2)
        x2t = carve2(DM)
        abuf = carve2(TF + 2)
        accb = carve2(TF)
        gfbc = carve2(DM)
        cwst = carve2(4 * 128)[0:NFC, :].rearrange("p (k t) -> p k t", k=4)

        gcol = SB("gcol", [128, 16], F32)
        gst = SB("gst", [8, 2, 128], F32)
        hb = SB("hb", [128, DM], BF16)
        junkb = hb
        hT = SB("hT", [128, 8, 128], BF16)
        mixT = hT
        ss = SB("ss", [128, 4], F32)
        W2_sb = SB("W2_sb", [128, 2, 64], BF16)
        posT = SB("posT", [128, 32], BF16)
        constc = SB("constc", [128, 2], F32)
        kcmpT = SB("kcmpT", [64, 256], BF16)
        Vc = SB("Vc", [128, 2, 129], BF16)
        zs = SB("zs", [128, 5, 32], F32)
        hidT = SB("hidT", [128, 32], BF16)
        vst = SB("vst", [8, 64], BF16)
        ropet = SB("ropet", [128, 4, 12, 8], F32)
        iqf = SB("iqf", [128, 9, 32], F32)
        iws = SB("iws", [128, 3, 8], F32)
        gs = SB("gs", [128, 15], F32)
        dens = SB("dens", [128, 15], F32)
        coef = SB("coef", [128, 15], F32)
        rdb = SB("rdb", [128, 16], F32)
        impf = SB("impf", [128, 2, 64], F32)
        fb = SB("fb", [128, 64], F32)
        m8 = SB("m8", [128, 16], F32)
        bmb = SB("bmb", [128, 64], BF16)
        bis = SB("bis", [128, 4], F32)
        carry = SB("carry", [128, NFC, 2], F32)
        cwall = SB("cwall", [128, 4, NFC], F32)
        identf = SB("identf", [128, 128], F32)

        P.dma("pool", lambda e: e.dma_start(out=ident[:], in_=c_ident[:, :]), writes=["ident"])
        P.dma("pool", lambda e: e.dma_start(out=i4[:], in_=c_i4[:, :]), writes=["i4"])
        P.dma("pool", lambda e: e.dma_start(out=biasT[:], in_=c_bias[:, :, :]), writes=["biasT"])
        P.dma("sp", lambda e: e.dma_start(out=csh[:], in_=c_csh[:, :, :]), writes=["csh"])
        P.dma("sp", lambda e: e.dma_start(out=csi[:], in_=c_csi[:, :, :]), writes=["csi"])
        P.dma("sp", lambda e: e.dma_start(out=identf[:], in_=c_ident[:, :]), writes=["identf"])
        P.op("pool", lambda e: e.memset(neghalf[:], -0.5), writes=["neghalf"])
        P.op("pool", lambda e: e.memset(zerosb[:], 0.0), writes=["zerosb"])

        sig = lambda e, out, in_: None

        def emit_layer(li, xin_ap, xout_ap, is_last):
            P.stage(1)
            P.barrier()
            for c in range(8):
                P.dma("pool", lambda e, c=c: e.dma_start(out=w_in_sb[:, c, :], in_=w_in[li, c * 128:(c + 1) * 128, :]), writes=["w_in_sb"])
            for c in range(8):
                P.dma("pool", lambda e, c=c: e.dma_start(out=w_out_sb[:, c, :], in_=w_outd[li, c * 128:(c + 1) * 128, :]), writes=["w_out_sb"])
            P.dma("sp", lambda e: e.dma_start(out=gst[:, 0, :], in_=w_norm1[li].rearrange("(c p) -> c p", p=128)), writes=["gst"])
            P.dma("sp", lambda e: e.dma_start(out=gst[:, 1, :], in_=w_norm2[li].rearrange("(c p) -> c p", p=128)), writes=["gst"])
            for k in range(2):
                P.op("pe", lambda e, k=k: e.transpose(out=psP[:, k * 8:(k + 1) * 8], in_=gst[:, k, :], identity=identf[0:8, 0:8]), reads=["gst", "identf"], writes=["psP"])
            P.op("dve", lambda e: e.tensor_copy(out=gcol[:], in_=psP[:, 0:16]), reads=["psP"], writes=["gcol"])
            for c in range(2):
                P.dma("pool", lambda e, c=c: e.dma_start(out=W1_sb[c * 64:(c + 1) * 64, :, :], in_=w_c1[li, c].rearrange("(j d) h -> d j h", d=64)), writes=["W1_sb"])
                P.dma("pool", lambda e, c=c: e.dma_start(out=W2_sb[:, c, :], in_=w_c2[li, c]), writes=["W2_sb"])
                P.dma("pool", lambda e, c=c: e.dma_start(out=posT[c * 64:(c + 1) * 64, :], in_=w_pos[li, c].rearrange("j d -> d j"), allow_slow_non_contiguous=True), writes=["posT"])
            P.dma("pool", lambda e: e.dma_start(out=Vc[:, :, 65:129], in_=c_ov[:, :, :]), writes=["Vc"])
            P.op("pool", lambda e: e.memset(Vc[:, :, 0:64], 0.0), writes=["Vc"])
            P.op("pool", lambda e: e.memset(Vc[:, :, 64:65], 1.0), writes=["Vc"])
            P.op("pool", lambda e: e.memset(kcmpT[:], 0.0), writes=["kcmpT"])
            P.op("pool", lambda e: e.memset(Vs_a[:, :, 64:65], 1.0), writes=["Vs_a_ones"])
            P.op("pool", lambda e: e.memset(Vw_a[:, :, 64:65], 1.0), writes=["Vw_a_ones"])
            P.op("pool", lambda e: e.memset(V_b[:, :, 64:65], 1.0), writes=["V_b_ones"])
            for g in range(3):
                P.op("pool", lambda e, g=g: e.memset(V_c[g][:, :, :, 64:65], 1.0), writes=["V_c_ones%d" % g])
            for c in range(2):
                for jj in range(32):
                    P.op("pe", lambda e, c=c, jj=jj: e.matmul((psP if c == 0 else psA)[:, 0:1], lhsT=W1_sb[c * 64:(c + 1) * 64, jj, :], rhs=posT[c * 64:(c + 1) * 64, jj:jj + 1],
                                                             start=(jj == 0), stop=(jj == 31), skip_group_check=True),
                         reads=["W1_sb", "posT"], writes=["psP" if c == 0 else "psA"])
            P.op("dve", lambda e: e.tensor_copy(out=constc[:, 0:1], in_=psP[:, 0:1]), reads=["psP"], writes=["constc"])
            P.op("dve", lambda e: e.tensor_copy(out=constc[:, 1:2], in_=psA[:, 0:1]), reads=["psA"], writes=["constc"])

            st_toggle = [0]

            def attention(name, H, W, items, qfn, Ops, Okeys, Osb, Osb_key):
                nitems = len(items)
                first_in_bank = {}

                def one_item(ii, ktf, vf, bias_ap, rkeys, shared):
                    b = st_toggle[0]
                    st_toggle[0] ^= 1
                    psX = psA if b == 0 else psB
                    pskey = "psA" if b == 0 else "psB"
                    ptb = PT[b]
                    ptkey = "PT%d" % b
                    nob = bias_ap is None
                    if shared:
                        h1 = min(H, 4)
                        P.op("pe", lambda e: e.matmul(psX[:, 0:h1 * 128], lhsT=ktf(0), rhs=qfn(0, h1), start=True, stop=nob, skip_group_check=True),
                             reads=rkeys + [name + "_Q"], writes=[pskey])
                        if H > 4:
                            P.op("pe", lambda e: e.matmul(psX[:, 512:512 + (H - 4) * 128], lhsT=ktf(0), rhs=qfn(4, H), start=True, stop=nob, skip_group_check=True),
                                 reads=rkeys + [name + "_Q"], writes=[pskey])
                    else:
                        for h in range(H):
                            P.op("pe", lambda e, h=h: e.matmul(psX[:, h * 512:h * 512 + 128], lhsT=ktf(h), rhs=qfn(h, h + 1), start=True, stop=nob, skip_group_check=True),
                                 reads=rkeys + [name + "_Q"], writes=[pskey])
                    if not nob and not shared:
                        for h in range(H):
                            P.op("pe", lambda e, h=h: e.matmul(psX[:, h * 512:h * 512 + 128], lhsT=bias_ap, rhs=i4[:, 0:128], start=False, stop=True, skip_group_check=True),
                                 reads=rkeys + ["i4"], writes=[pskey])
                    if not nob and shared:
                        h1 = min(H, 4)
                        P.op("pe", lambda e: e.matmul(psX[:, 0:h1 * 128], lhsT=bias_ap, rhs=i4[:, 0:h1 * 128], start=False, stop=True, skip_group_check=True),
                             reads=rkeys + ["i4"], writes=[pskey])
                        if H > 4:
                            P.op("pe", lambda e: e.matmul(psX[:, 512:512 + (H - 4) * 128], lhsT=bias_ap, rhs=i4[:, 0:(H - 4) * 128], start=False, stop=True, skip_group_check=True),
                                 reads=rkeys + ["i4"], writes=[pskey])
                    if shared:
                        P.op("act", lambda e: e.activation(out=ptb[:, 0:H * 128], in_=psX[:, 0:H * 128], func=AF.Exp, scale=SCALE),
                             reads=[pskey], writes=[ptkey])
                    else:
                        P.op("act", lambda e: e.activation(out=ptb[:, 0:H * 128].rearrange("p (h t) -> p h t", h=H), in_=psX[:, :].rearrange("p (h c) -> p h c", h=2)[:, 0:H, 0:128], func=AF.Exp, scale=SCALE),
                             reads=[pskey], writes=[ptkey])
                    for h in range(H):
                        oap, okey = Ops(h)
                        fst = okey not in first_in_bank
                        first_in_bank[okey] = True
                        P.op("pe", lambda e, oap=oap, h=h, fst=fst: e.matmul(oap, lhsT=ptb[:, h * 128:(h + 1) * 128], rhs=vf(h), start=fst, stop=(ii == nitems - 1), skip_group_check=True),
                             reads=[ptkey] + rkeys, writes=[okey])

                for ii, (ktf, vf, bias_ap, rkeys, shared) in enumerate(items):
                    one_item(ii, ktf, vf, bias_ap, rkeys, shared)

            def tile_body(n):
                xb_ = xt[n % 2]
                xk = "xt"
                T0 = n * 128
                P.stage(2)
                P.dma("sp", lambda e, xb_=xb_: e.dma_start(out=xb_[:], in_=xin_ap[T0:T0 + 128, :]), writes=[xk])
                P.op("dve", lambda e, xb_=xb_: e.scalar_tensor_tensor(out=junkb[:], in0=xb_[:], scalar=1.0, in1=xb_[:], op0=ALU.mult, op1=ALU.mult, accum_out=ss[:, 0:1]),
                     reads=[xk], writes=["hb", "ss0"])
                P.op("dve", lambda e: e.tensor_scalar(out=ss[:, 1:2], in0=ss[:, 0:1], scalar1=1.0 / DM, scalar2=EPS, op0=ALU.mult, op1=ALU.add), reads=["ss0"], writes=["ss1"])
                P.op("pool!", lambda e: e.tensor_tensor(out=ss[:, 2:3], in0=ss[:, 1:2], in1=neghalf[:], op=ALU.pow), reads=["ss1", "neghalf"], writes=["ss2"])
                P.op("dve", lambda e, xb_=xb_: e.tensor_scalar(out=hb[:], in0=xb_[:], scalar1=ss[:, 2:3], scalar2=None, op0=ALU.mult),
                     reads=[xk, "ss2"], writes=["hb"])
                for c in range(8):
                    P.op("pe", lambda e, c=c: e.transpose(out=psT[:, c * 128:(c + 1) * 128], in_=hb[:, c * 128:(c + 1) * 128], identity=ident[:]), reads=["hb", "ident"], writes=["psT"])
                P.op("dve", lambda e: e.tensor_tensor(out=hT[:], in0=psT[:, :].rearrange("p (c t) -> p c t", c=8), in1=gcol[:, 0:8].unsqueeze(2).to_broadcast([128, 8, 128]), op=ALU.mult),
                     reads=["psT", "gcol"], writes=["hT"])
                P.stage(3)
                ngrp = (DIN + 511) // 512
                for gi in range(ngrp):
                    c0 = gi * 512
                    c1 = min(DIN, c0 + 512)
                    psX = psA if gi % 2 == 0 else psB
                    pk = "psA" if gi % 2 == 0 else "psB"
                    for c in range(8):
                        P.op("pe", lambda e, c=c, c0=c0, c1=c1, psX=psX: e.matmul(psX[:, 0:c1 - c0], lhsT=hT[:, c, :], rhs=w_in_sb[:, c, c0:c1], start=(c == 0), stop=(c == 7)),
                             reads=["hT", "w_in_sb"], writes=[pk])
                    if gi % 2 == 0:
                        P.op("act", lambda e, c0=c0, c1=c1, psX=psX: e.copy(out=Pf[:, c0:c1], in_=psX[:, 0:c1 - c0]), reads=[pk], writes=["Pf"])
                    else:
                        P.op("dve", lambda e, c0=c0, c1=c1, psX=psX: e.tensor_copy(out=Pf[:, c0:c1], in_=psX[:, 0:c1 - c0]), reads=[pk], writes=["Pf"])
                if dbg and li == 0:
                    P.dma("sp", lambda e: e.dma_start(out=dbg_out["d_pf"][T0:T0 + 128, :], in_=Pf[:]), reads=["Pf"], writes=["d_pf"])

                P.stage(4)
                def rope_set(src_ap, H, dst_ap, tab, half, D, dup=False, key="Tb"):
                    cosb = tab[:, n, 0:half].unsqueeze(1).to_broadcast([128, H, half])
                    sinb = tab[:, n, half:2 * half].unsqueeze(1).to_broadcast([128, H, half])
                    x1 = src_ap[:, :, 0:half]
                    x2 = src_ap[:, :, half:2 * half]
                    t = [ropet[:, k, 0:H, 0:half] for k in range(4)]
                    rk = ["Pf", "csh", "csi"]
                    P.op("dve", lambda e: e.tensor_tensor(out=t[0], in0=x1, in1=cosb, op=ALU.mult), reads=rk, writes=["ropet0"])
                    P.op("dve", lambda e: e.tensor_tensor(out=t[1], in0=x2, in1=sinb, op=ALU.mult), reads=rk, writes=["ropet1"])
                    P.op("dve", lambda e: e.tensor_tensor(out=t[2], in0=x2, in1=cosb, op=ALU.mult), reads=rk, writes=["ropet2"])
                    P.op("dve", lambda e: e.tensor_tensor(out=t[3], in0=x1, in1=sinb, op=ALU.mult), reads=rk, writes=["ropet3"])
                    dsts = [dst_ap[:, :, 0, :], dst_ap[:, :, 1, :]] if dup else [dst_ap]
                    for d_ in dsts:
                        P.op("dve", lambda e, d_=d_: e.tensor_tensor(out=d_[:, :, 0:half], in0=t[0], in1=t[1], op=ALU.subtract), reads=["ropet0", "ropet1"], writes=[key])
                        P.op("dve", lambda e, d_=d_: e.tensor_tensor(out=d_[:, :, half:2 * half], in0=t[2], in1=t[3], op=ALU.add), reads=["ropet2", "ropet3"], writes=[key])
                        P.op("pool", lambda e, d_=d_: e.tensor_copy(out=d_[:, :, 2 * half:D], in_=src_ap[:, :, 2 * half:D]), reads=["Pf"], writes=[key])

                P.stage(4.05)
                P.op("pool", lambda e: e.tensor_copy(out=Tb[:, TB_AQ:TB_AQ + 320], in_=Pf[:, O_AQ:O_AQ + 320]), reads=["Pf"], writes=["Tb"])
                P.op("pool", lambda e: e.tensor_copy(out=Tb[:, TB_KCV:TB_KCV + 128], in_=Pf[:, O_AKC:O_AKC + 128]), reads=["Pf"], writes=["Tb"])
                P.stage(4.1)
                rope_set(Pf[:, O_AQ:O_AQ + 320].rearrange("p (h d) -> p h d", d=64), 5,
                         Tb[:, TB_AQR:TB_AQR + 640].rearrange("p (h u d) -> p h u d", u=2, d=64), csh, 8, 64, dup=True)
                rope_set(Pf[:, O_AKS:O_AKS + 256].rearrange("p (h d) -> p h d", d=128)[:, :, 0:64], 2,
                         Tb[:, TB_KA:TB_KA + 128].rearrange("p (h d) -> p h d", d=64), csh, 8, 64)
                rope_set(Pf[:, O_BQ:O_BQ + 384].rearrange("p (h d) -> p h d", d=64), 6,
                         Tb[:, TB_B:TB_B + 384].rearrange("p (h d) -> p h d", d=64), csh, 8, 64)
                rope_set(Pf[:, O_CQ:O_CQ + 768].rearrange("p (h d) -> p h d", d=64), 12,
                         Tb[:, TB_C:TB_C + 768].rearrange("p (h d) -> p h d", d=64), csh, 8, 64)
                P.stage(4.2)
                rope_set(Pf[:, O_BIQ:O_BIQ + 288].rearrange("p (h d) -> p h d", d=32), 9, iqf[:], csi, 4, 32, key="iqf")
                P.stage(4.3)
                cidx = 1.0 / math.sqrt(32.0) / math.sqrt(8.0)
                P.op("dve", lambda e: e.tensor_scalar(out=iws[:, 2, :], in0=Pf[:, O_BIW:O_BIW + 8], scalar1=-1.0, scalar2=None, op0=ALU.mult), reads=["Pf"], writes=["iws2"])
                P.op("dve", lambda e: e.tensor_tensor(out=iws[:, 0, :], in0=iws[:, 2, :], in1=Pf[:, O_BIW:O_BIW + 8], op=ALU.max), reads=["Pf", "iws2"], writes=["iws0"])
                P.op("dve", lambda e: e.tensor_scalar(out=iws[:, 0, :], in0=iws[:, 0, :], scalar1=cidx, scalar2=None, op0=ALU.mult), reads=["iws0"], writes=["iws0"])
                P.op("dve", lambda e: e.tensor_scalar(out=iws[:, 2, :], in0=Pf[:, O_BIW:O_BIW + 8], scalar1=0.0, scalar2=2.0, op0=ALU.is_ge, op1=ALU.mult), reads=["Pf"], writes=["iws2"])
                P.op("dve", lambda e: e.tensor_scalar(out=iws[:, 1, :], in0=iws[:, 2, :], scalar1=-1.0, scalar2=None, op0=ALU.add), reads=["iws2"], writes=["iws1"])
                P.op("dve", lambda e: e.tensor_tensor(out=Tb[:, TB_QI:TB_QI + 256].rearrange("p (h d) -> p h d", d=32), in0=iqf[:, 0:8, :],
                                                      in1=iws[:, 0, :].unsqueeze(2).to_broadcast([128, 8, 32]), op=ALU.mult), reads=["iqf", "iws0"], writes=["Tb"])
                P.stage(4.4)
                P.op("pool", lambda e: e.tensor_copy(out=Tb[:, TB_KI:TB_KI + 128].rearrange("p (r d) -> p r d", d=32), in_=iqf[:, 8:9, :].to_broadcast([128, 4, 32])), reads=["iqf"], writes=["Tb"])
                P.stage(4.5)
                for h in range(8):
                    P.op("pool", lambda e, h=h: e.tensor_scalar(out=Dh[:, h, :], in0=ident[:], scalar1=iws[:, 1, h:h + 1], scalar2=None, op0=ALU.mult), reads=["ident", "iws1"], writes=["Dh"])
                P.stage(4.6)
                P.op("act", lambda e: e.activation(out=gs[:], in_=Pf[:, O_AG:O_AG + 15], func=AF.Exp, scale=-1.0), reads=["Pf"], writes=["gs"])
                P.op("dve", lambda e: e.tensor_scalar(out=gs[:], in0=gs[:], scalar1=1.0, scalar2=None, op0=ALU.add), reads=["gs"], writes=["gs"])
                P.op("dve", lambda e: e.reciprocal(out=gs[:], in_=gs[:]), reads=["gs"], writes=["gs"])
                P.stage(4.7)
                P.op("pool", lambda e: e.tensor_copy(out=Vs_a[:, n, 0:64], in_=Pf[:, O_AVS:O_AVS + 64]), reads=["Pf"], writes=["Vs_a:%d" % n])
                P.op("pool", lambda e: e.tensor_copy(out=Vw_a[:, n % 5, 0:64], in_=Pf[:, O_AVW:O_AVW + 64]), reads=["Pf"], writes=["Vw_a:%d" % (n % 5)])
                P.op("pool", lambda e: e.tensor_copy(out=V_b[:, n, 0:64], in_=Pf[:, O_BV:O_BV + 64]), reads=["Pf"], writes=["V_b:%d" % n])
                for g in range(3):
                    sl = n % DIL_R[g]
                    P.op("pool", lambda e, g=g, sl=sl: e.tensor_copy(out=V_c[g][:, sl, :, 0:64], in_=Pf[:, O_CV + g * 128:O_CV + (g + 1) * 128].rearrange("p (h d) -> p h d", d=64)),
                         reads=["Pf"], writes=["V_c%d:%d" % (g, sl)])

                P.stage(5)
                for h in range(5):
                    P.op("pe", lambda e, h=h: e.transpose(out=psT[0:64, h * 128:(h + 1) * 128], in_=Tb[:, TB_AQ + h * 64:TB_AQ + (h + 1) * 64], identity=ident[:]), reads=["Tb", "ident"], writes=["psT"])
                P.op("act", lambda e: e.copy(out=QT_araw[0:64, :], in_=psT[0:64, 0:640]), reads=["psT"], writes=["cmp_Q"])
                P.stage(5.1)
                for h in range(5):
                    P.op("pe", lambda e, h=h: e.transpose(out=psT[:, h * 128:(h + 1) * 128], in_=Tb[:, TB_AQR + h * 128:TB_AQR + (h + 1) * 128], identity=ident[:]), reads=["Tb", "ident"], writes=["psT"])
                P.op("dve", lambda e: e.tensor_copy(out=QT_ar[:], in_=psT[:, 0:640]), reads=["psT"], writes=["slc_Q", "win_Q"])
                P.stage(5.2)
                P.stage(5.2 + 0.01 * 1)
                P.op("pe", lambda e: e.transpose(out=psT[:, 0:128], in_=Tb[:, TB_KA:TB_KA + 128], identity=ident[:]), reads=["Tb", "ident"], writes=["psT"])
                P.stage(5.2 + 0.01 * 2)
                P.op("pe", lambda e: e.transpose(out=psT[:, 128:256], in_=Tb[:, TB_KCV:TB_KCV + 128], identity=ident[:]), reads=["Tb", "ident"], writes=["psT"])
                P.stage(5.2 + 0.01 * 3)
                P.op("pe", lambda e: e.transpose(out=psT[:, 256:384], in_=Tb[:, TB_QI:TB_QI + 128], identity=ident[:]), reads=["Tb", "ident"], writes=["psT"])
                P.stage(5.2 + 0.01 * 4)
                P.op("pe", lambda e: e.transpose(out=psT[:, 384:512], in_=Tb[:, TB_QI + 128:TB_QI + 256], identity=ident[:]), reads=["Tb", "ident"], writes=["psT"])
                P.stage(5.2 + 0.01 * 5)
                P.op("pe", lambda e: e.transpose(out=psT[:, 512:640], in_=Tb[:, TB_KI:TB_KI + 128], identity=ident[:]), reads=["Tb", "ident"], writes=["psT"])
                P.stage(5.2 + 0.01 * 6)
                P.op("act", lambda e: e.copy(out=KT_a[:, T0:T0 + 128], in_=psT[:, 0:128]), reads=["psT"], writes=["KT_a:%d" % n])
                P.stage(5.2 + 0.01 * 7)
                P.op("act", lambda e: e.copy(out=kcvT[:, T0:T0 + 128], in_=psT[:, 128:256]), reads=["psT"], writes=["kcvT:%d" % n])
                P.stage(5.2 + 0.01 * 8)
                P.op("dve", lambda e: e.tensor_copy(out=qiT[:].rearrange("p a t -> p (a t)"), in_=psT[:, 256:512]), reads=["psT"], writes=["qiT"])
                P.stage(5.2 + 0.01 * 9)
                P.op("dve", lambda e: e.tensor_copy(out=kiT4[:, T0:T0 + 128], in_=psT[:, 512:640]), reads=["psT"], writes=["kiT4:%d" % n])
                P.stage(5.3)
                for h in range(6):
                    P.op("pe", lambda e, h=h: e.transpose(out=psT[0:64, h * 128:(h + 1) * 128], in_=Tb[:, TB_B + h * 64:TB_B + (h + 1) * 64], identity=ident[:]), reads=["Tb", "ident"], writes=["psT"])
                P.op("act", lambda e: e.copy(out=QT_b[0:64, :], in_=psT[0:64, 0:640]), reads=["psT"], writes=["dsa_Q"])
                P.op("dve", lambda e: e.tensor_copy(out=KT_b[0:64, T0:T0 + 128], in_=psT[0:64, 640:768]), reads=["psT"], writes=["KT_b:%d" % n])
                P.stage(5.4)
                for k in range(6):
                    P.op("pe", lambda e, k=k: e.transpose(out=psT[:, k * 128:(k + 1) * 128], in_=Tb[:, TB_C + k * 128:TB_C + (k + 1) * 128], identity=ident[:]), reads=["Tb", "ident"], writes=["psT"])
                P.op("act", lambda e: e.copy(out=QT_c[:].rearrange("p a t -> p (a t)"), in_=psT[:, 0:384]), reads=["psT"], writes=["dil0_Q", "dil1_Q", "dil2_Q"])
                for g in range(3):
                    sl = n % DIL_R[g]
                    P.op("dve", lambda e, g=g, sl=sl: e.tensor_copy(out=KT_c[g][:, sl, :], in_=psT[:, 384 + g * 128:384 + (g + 1) * 128]), reads=["psT"], writes=["KT_c%d:%d" % (g, sl)])

                P.stage(6)
                j0 = 0 if n == 0 else 8 * n - 1
                nb = 7 if n == 0 else 8
                if n == NT - 1:
                    nb = 8
                tk0 = 16 * j0
                rk_kcv = ["kcvT:%d" % n] + (["kcvT:%d" % (n - 1)] if n > 0 else [])
                for c in range(2):
                    for jj in range(32):
                        a0 = tk0 + jj
                        P.op("pe", lambda e, c=c, jj=jj, a0=a0: e.matmul((psP if c == 0 else psA)[:, 0:nb], lhsT=W1_sb[c * 64:(c + 1) * 64, jj, :],
                                                                          rhs=kcvT[c * 64:(c + 1) * 64, a0:a0 + 16 * (nb - 1) + 1:16],
                                                                          start=(jj == 0), stop=(jj == 31), skip_group_check=True),
                             reads=["W1_sb"] + rk_kcv, writes=["psP" if c == 0 else "psA"])
                for c in range(2):
                    P.op("dve", lambda e, c=c: e.tensor_scalar(out=zs[:, 0, c * 16:c * 16 + nb], in0=(psP if c == 0 else psA)[:, 0:nb], scalar1=constc[:, c:c + 1], scalar2=None, op0=ALU.add),
                         reads=["psP" if c == 0 else "psA", "constc"], writes=["zs0"])
                if nb < 16:
                    pass
                zsl = lambda k: zs[:, k, :].rearrange("p (c b) -> p c b", c=2)[:, :, 0:nb]
                P.op("dve", lambda e: e.tensor_tensor(out=zsl(1), in0=zsl(0), in1=zsl(0), op=ALU.mult), reads=["zs0"], writes=["zs1"])
                P.op("dve", lambda e: e.tensor_scalar(out=zsl(1), in0=zsl(1), scalar1=0.044715, scalar2=1.0, op0=ALU.mult, op1=ALU.add), reads=["zs1"], writes=["zs1"])
                P.op("dve", lambda e: e.tensor_tensor(out=zsl(2), in0=zsl(1), in1=zsl(0), op=ALU.mult), reads=["zs0", "zs1"], writes=["zs2"])
                P.op("act", lambda e: e.activation(out=zsl(3), in_=zsl(2), func=AF.Tanh, scale=0.7978845608028654), reads=["zs2"], writes=["zs3"])
                P.op("dve", lambda e: e.tensor_scalar(out=zsl(3), in0=zsl(3), scalar1=1.0, scalar2=0.5, op0=ALU.add, op1=ALU.mult), reads=["zs3"], writes=["zs3"])
                P.op("dve", lambda e: e.tensor_tensor(out=hidT[:].rearrange("p (c b) -> p c b", c=2)[:, :, 0:nb], in0=zsl(3), in1=zsl(0), op=ALU.mult), reads=["zs3", "zs0"], writes=["hidT"])
                P.op("pe", lambda e: e.matmul(psP[0:64, 32:32 + nb], lhsT=W2_sb[:, 0, :], rhs=hidT[:, 0:nb], start=True, stop=True, skip_group_check=True), reads=["W2_sb", "hidT"], writes=["psP"])
                P.op("pe", lambda e: e.matmul(psP[0:nb, 64:128], lhsT=hidT[:, 16:16 + nb], rhs=W2_sb[:, 1, :], start=True, stop=True, skip_group_check=True), reads=["W2_sb", "hidT"], writes=["psP"])
                P.op("dve", lambda e: e.tensor_copy(out=kcmpT[:, j0:j0 + nb], in_=psP[0:64, 32:32 + nb]), reads=["psP"], writes=["kcmpT"])
                P.op("dve", lambda e: e.tensor_copy(out=vst[0:nb, :], in_=psP[0:nb, 64:128]), reads=["psP"], writes=["vst"])
                b = 0
                while b < nb:
                    j = j0 + b
                    tl, pp = j // 128, j % 128
                    cnt = min(nb - b, 128 - pp)
                    P.dma("sp", lambda e, b=b, tl=tl, pp=pp, cnt=cnt: e.dma_start(out=Vc[pp:pp + cnt, tl, 0:64], in_=vst[b:b + cnt, :]), reads=["vst"], writes=["Vc"])
                    b += cnt

                S = (n + 1) * 128
                P.stage(7)
                nch = (S + 511) // 512
                for ch in range(nch):
                    k0 = ch * 512
                    wc = min(512, S - k0)
                    rkk = ["kiT4:%d" % kt for kt in range(k0 // 128, (k0 + wc) // 128)]
                    for half in range(2):
                        for j in range(4):
                            dst = (psA if j < 2 else psB)[:, (j % 2) * 512:(j % 2) * 512 + wc]
                            dk = "psA" if j < 2 else "psB"
                            P.op("pe", lambda e, j=j, half=half, dst=dst, k0=k0, wc=wc: e.matmul(dst, lhsT=qiT[j * 32:(j + 1) * 32, half, :], rhs=kiT4[j * 32:(j + 1) * 32, k0:k0 + wc],
                                                                                                  start=True, stop=True, tile_position=(j * 32, 0), skip_group_check=True),
                                 reads=["qiT"] + rkk, writes=[dk])
                        for j in range(4):
                            src = (psA if j < 2 else psB)[:, (j % 2) * 512:(j % 2) * 512 + wc]
                            dk = "psA" if j < 2 else "psB"
                            if j % 2 == 0:
                                P.op("act", lambda e, j=j, src=src, wc=wc: e.activation(out=Rbuf[:, j, 0:wc], in_=src, func=AF.Relu), reads=[dk], writes=["R%d" % j])
                            else:
                                P.op("dve", lambda e, j=j, src=src, wc=wc: e.tensor_scalar(out=Rbuf[:, j, 0:wc], in0=src, scalar1=0.0, scalar2=None, op0=ALU.max), reads=[dk], writes=["R%d" % j])
                        for j in range(4):
                            h = half * 4 + j
                            P.op("pe", lambda e, h=h, j=j, wc=wc, ch=ch: e.matmul(psP[:, 0:wc], lhsT=Dh[:, h, :], rhs=Rbuf[:, j, 0:wc], start=(h == 0), stop=(h == 7 and ch != nch - 1), skip_group_check=True),
                                 reads=["Dh", "R%d" % j], writes=["psP"])
                    if ch == nch - 1:
                        P.op("pe", lambda e, wc=wc: e.matmul(psP[:, wc - 128:wc], lhsT=ident[:], rhs=biasT[:, NB_BIAS - 1, :], start=False, stop=True, skip_group_check=True),
                             reads=["ident", "biasT"], writes=["psP"])
                    P.op("act", lambda e, k0=k0, wc=wc: e.copy(out=score[:, k0:k0 + wc], in_=psP[:, 0:wc]), reads=["psP"], writes=["score"])
                P.stage(8)
                P.op("pool", lambda e: e.memset(bis[:, 0:1], BIS_LO), writes=["bis_lo"])
                if n >= 2:
                    w = -BIS_LO
                    P.op("pool", lambda e: e.memset(bis[:, 1:2], 0.0), writes=["bis_mid"])
                    for it in range(BIS_ITERS):
                        P.op("dve", lambda e: e.tensor_scalar(out=mb_d[:, 0:S], in0=score[:, 0:S], scalar1=bis[:, 1:2], scalar2=None, op0=ALU.is_ge, op1=ALU.add, accum_out=bis[:, 2:3]),
                             reads=["score", "bis_mid"], writes=["mb", "bis_cnt"])
                        P.op("dve", lambda e, w=w: e.tensor_scalar(out=bis[:, 3:4], in0=bis[:, 2:3], scalar1=255.5, scalar2=w, op0=ALU.is_ge, op1=ALU.mult), reads=["bis_cnt"], writes=["bis_g"])
                        P.op("dve", lambda e: e.tensor_tensor(out=bis[:, 0:1], in0=bis[:, 0:1], in1=bis[:, 3:4], op=ALU.add), reads=["bis_lo", "bis_g"], writes=["bis_lo"])
                        w = w / 2.0
                        P.op("dve", lambda e, w=w: e.tensor_scalar(out=bis[:, 1:2], in0=bis[:, 0:1], scalar1=w, scalar2=None, op0=ALU.add), reads=["bis_lo"], writes=["bis_mid"])
                P.op("dve", lambda e: e.tensor_scalar(out=mb_d[:, 0:S], in0=score[:, 0:S], scalar1=bis[:, 0:1], scalar2=MASKV, op0=ALU.is_lt, op1=ALU.mult), reads=["score", "bis_lo"], writes=["mb"])
                P.stage(9)
                items = [(lambda h, kt=kt: KT_b[0:64, kt * 128:(kt + 1) * 128], lambda h, kt=kt: V_b[:, kt, :], mb_d[:, kt * 128:(kt + 1) * 128],
                          ["KT_b:%d" % kt, "V_b:%d" % kt, "V_b_ones", "mb"], True) for kt in range(n + 1)]
                attention("dsa", 5, 65, items, lambda h0, h1: QT_b[0:64, h0 * 128:h1 * 128], lambda h: (psO[:, h * 65:(h + 1) * 65], "psO0"), None, None, None)
                P.op("act", lambda e: e.copy(out=Odsa[:].rearrange("p h w -> p (h w)"), in_=psO[:, 0:325]), reads=["psO0"], writes=["Odsa", "score"])
                P.stage(10)
                items = []
                for nt_ in range(2):
                    base = 128 * n - 2048 * nt_ - 31
                    if base + 127 < 0:
                        continue
                    bias_ap = None
                    if base < 16 * 127:
                        cbt = mb_s[:, nt_ * 128:(nt_ + 1) * 128]
                        ci = cmp_pairs.index((n, nt_))
                        P.dma("pool", lambda e, cbt=cbt, ci=ci: e.dma_start(out=cbt, in_=c_cmpb[ci]), writes=["mb"])
                        bias_ap = cbt
                    items.append((lambda h, nt_=nt_: kcmpT[:, nt_ * 128:(nt_ + 1) * 128], lambda h, nt_=nt_: Vc[:, nt_, :], bias_ap, ["kcmpT", "Vc", "mb"], True))

                def ops_cmp(h):
                    bk = h // 3
                    return psO[:, bk * 512 + (h % 3) * 129: bk * 512 + (h % 3 + 1) * 129], "psO%d" % bk
                P.stage(10.1)
                attention("cmp", 5, 129, items, lambda h0, h1: QT_araw[0:64, h0 * 128:h1 * 128], ops_cmp, None, None, None)
                P.stage(10.2)
                P.op("act", lambda e: e.copy(out=Ocmp[:, 0:3, :].rearrange("p h w -> p (h w)"), in_=psO[:, 0:387]), reads=["psO0"], writes=["Ocmp", "score"])
                P.op("act", lambda e: e.copy(out=Ocmp[:, 3:5, :].rearrange("p h w -> p (h w)"), in_=psO[:, 512:512 + 258]), reads=["psO1"], writes=["Ocmp", "score"])
                P.stage(10.3)
                P.op("dve", lambda e: e.tensor_scalar(out=rdb[:, 0:5], in0=Ocmp[:, :, 64], scalar1=1e-30, scalar2=None, op0=ALU.max), reads=["Ocmp"], writes=["rdb"])
                P.op("dve", lambda e: e.reciprocal(out=rdb[:, 0:5], in_=rdb[:, 0:5]), reads=["rdb"], writes=["rdb"])
                P.stage(10.4)
                P.op("pool", lambda e: e.memset(fb[:], 0.0), writes=["fb"])
                if 2 * n + 1 < 64:
                    P.op("pool", lambda e: e.memset(fb[0:64, 2 * n + 1:64], NEGBIG), writes=["fb"])
                if 2 * n + 2 < 64:
                    P.op("pool", lambda e: e.memset(fb[64:128, 2 * n + 2:64], NEGBIG), writes=["fb"])
                P.op("pool", lambda e: e.memset(fb[:, 0:1], 1e9), writes=["fb"])
                P.op("pool", lambda e: e.memset(fb[0:64, 2 * n:2 * n + 1], 1e9), writes=["fb"])
                if n >= 1:
                    P.op("pool", lambda e: e.memset(fb[0:64, 2 * n - 1:2 * n], 1e9), writes=["fb"])
                P.op("pool", lambda e: e.memset(fb[64:128, 2 * n:2 * n + 2], 1e9), writes=["fb"])
                P.stage(10.5)
                for h in range(5):
                    P.op("dve", lambda e, h=h: e.scalar_tensor_tensor(out=impf[:, 0, :], in0=Ocmp[:, h, 65:129], scalar=rdb[:, h:h + 1], in1=(fb[:] if h == 0 else impf[:, 0, :]), op0=ALU.mult, op1=ALU.add),
                         reads=["Ocmp", "rdb", "fb", "impf0"], writes=["impf0"])
                P.stage(10.6)
                P.op("dve", lambda e: e.max(out=m8[:, 0:8], in_=impf[:, 0, :]), reads=["impf0"], writes=["m8a"])
                P.op("dve", lambda e: e.match_replace(out=impf[:, 1, :], in_to_replace=m8[:, 0:8], in_values=impf[:, 0, :], imm_value=-3.0e38), reads=["m8a", "impf0"], writes=["impf1"])
                P.op("dve", lambda e: e.max(out=m8[:, 8:16], in_=impf[:, 1, :]), reads=["impf1"], writes=["m8b"])
                P.op("dve", lambda e: e.tensor_scalar(out=m8[:, 0:1], in0=m8[:, 15:16], scalar1=-1e29, scalar2=None, op0=ALU.max), reads=["m8b", "m8a"], writes=["m8a"])
                P.op("dve", lambda e: e.tensor_scalar(out=bmb[:], in0=impf[:, 0, :], scalar1=m8[:, 0:1], scalar2=MASKV, op0=ALU.is_lt, op1=ALU.mult), reads=["impf0", "m8a"], writes=["bmb"])
                P.stage(10.7)
                S = (n + 1) * 128
                nbk = S // 64
                P.op("dve", lambda e: e.tensor_copy(out=mb_s[:, 0:S].rearrange("p (j k) -> p j k", k=64), in_=bmb[:, 0:nbk].unsqueeze(2).to_broadcast([128, nbk, 64])), reads=["bmb"], writes=["mb"])
                P.op("dve", lambda e: e.tensor_tensor(out=mb_s[:, T0:T0 + 128], in0=mb_s[:, T0:T0 + 128], in1=biasT[:, 0, :], op=ALU.min), reads=["mb", "biasT"], writes=["mb"])

                P.stage(11)
                items = [(lambda h, kt=kt: KT_a[0:64, kt * 128:(kt + 1) * 128], lambda h, kt=kt: Vs_a[:, kt, :], mb_s[:, kt * 128:(kt + 1) * 128],
                          ["KT_a:%d" % kt, "Vs_a:%d" % kt, "Vs_a_ones", "mb"], True) for kt in range(n + 1)]
                attention("slc", 5, 65, items, lambda h0, h1: QT_ar[0:64, h0 * 128:h1 * 128], lambda h: (psO[:, h * 65:(h + 1) * 65], "psO0"), None, None, None)
                P.op("act", lambda e: e.copy(out=Oslc[:].rearrange("p h w -> p (h w)"), in_=psO[:, 0:325]), reads=["psO0"], writes=["Oslc", "score"])
                items = []
                for kt in range(max(0, n - 4), n + 1):
                    bias_ap = biasT[:, 0, :] if kt == n else (biasT[:, 1, :] if kt == n - 4 else None)
                    items.append((lambda h, kt=kt: KT_a[64:128, kt * 128:(kt + 1) * 128], lambda h, kt=kt: Vw_a[:, kt % 5, :], bias_ap,
                                  ["KT_a:%d" % kt, "Vw_a:%d" % (kt % 5), "Vw_a_ones", "biasT"], True))
                attention("win", 5, 65, items, lambda h0, h1: QT_ar[64:128, h0 * 128:h1 * 128], lambda h: (psO[:, 512 + h * 65:512 + (h + 1) * 65], "psO1"), None, None, None)
                P.op("act", lambda e: e.copy(out=Owin[:].rearrange("p h w -> p (h w)"), in_=psO[:, 512:512 + 325]), reads=["psO1"], writes=["Owin", "score"])

                P.stage(12)
                for g in range(3):
                    items = []
                    for o in range(DIL_O[g] + 1):
                        kt = n - o
                        if kt < 0:
                            continue
                        sl = kt % DIL_R[g]
                        items.append((lambda h, g=g, sl=sl: KT_c[g][h * 64:(h + 1) * 64, sl, :], lambda h, g=g, sl=sl: V_c[g][:, sl, h, :], biasT[:, _dil_bias_idx(g, o), :],
                                      ["KT_c%d:%d" % (g, sl), "V_c%d:%d" % (g, sl), "V_c_ones%d" % g, "biasT"], False))
                    bk = (g + 1) % 2
                    attention("dil%d" % g, 2, 65, items, lambda h0, h1, g=g: QT_c[h0 * 64:(h0 + 1) * 64, g, :],
                              lambda h, bk=bk: (psO[:, bk * 512 + h * 65:bk * 512 + (h + 1) * 65], "psO%d" % bk), None, None, None)
                    P.op("act", lambda e, g=g, bk=bk: e.copy(out=Odil[:, g, :, :].rearrange("p h w -> p (h w)"), in_=psO[:, bk * 512:bk * 512 + 130]), reads=["psO%d" % bk], writes=["Odil%d" % g, "score"])

                P.stage(13)
                dv = dens[:].rearrange("p (h b) -> p h b", b=3)
                P.op("dve", lambda e: e.tensor_copy(out=dv[:, :, 0], in_=Ocmp[:, :, 64]), reads=["Ocmp"], writes=["dens"])
                P.op("dve", lambda e: e.tensor_copy(out=dv[:, :, 1], in_=Oslc[:, :, 64]), reads=["Oslc"], writes=["dens"])
                P.op("dve", lambda e: e.tensor_copy(out=dv[:, :, 2], in_=Owin[:, :, 64]), reads=["Owin"], writes=["dens"])
                P.op("dve", lambda e: e.tensor_scalar(out=dens[:], in0=dens[:], scalar1=1e-30, scalar2=None, op0=ALU.max), reads=["dens"], writes=["dens"])
                P.op("dve", lambda e: e.reciprocal(out=dens[:], in_=dens[:]), reads=["dens"], writes=["dens"])
                P.op("dve", lambda e: e.tensor_tensor(out=coef[:], in0=dens[:], in1=gs[:], op=ALU.mult), reads=["dens", "gs"], writes=["coef"])
                cv = coef[:].rearrange("p (h b) -> p h b", b=3)
                P.op("dve", lambda e: e.tensor_tensor(out=tmpo[:, 0], in0=Ocmp[:, :, 0:64], in1=cv[:, :, 0:1].to_broadcast([128, 5, 64]), op=ALU.mult), reads=["Ocmp", "coef"], writes=["tmpo0"])
                P.op("dve", lambda e: e.tensor_tensor(out=tmpo[:, 1], in0=Oslc[:, :, 0:64], in1=cv[:, :, 1:2].to_broadcast([128, 5, 64]), op=ALU.mult), reads=["Oslc", "coef"], writes=["tmpo1"])
                P.op("dve", lambda e: e.tensor_tensor(out=tmpo[:, 0], in0=tmpo[:, 0], in1=tmpo[:, 1], op=ALU.add), reads=["tmpo0", "tmpo1"], writes=["tmpo0"])
                P.op("dve", lambda e: e.tensor_tensor(out=tmpo[:, 1], in0=Owin[:, :, 0:64], in1=cv[:, :, 2:3].to_broadcast([128, 5, 64]), op=ALU.mult), reads=["Owin", "coef"], writes=["tmpo1"])
                P.op("dve", lambda e: e.tensor_tensor(out=mixb[:, 0:320].rearrange("p (h d) -> p h d", d=64), in0=tmpo[:, 0], in1=tmpo[:, 1], op=ALU.add), reads=["tmpo0", "tmpo1"], writes=["mixb"])
                P.op("dve", lambda e: e.tensor_scalar(out=rdb[:, 8:13], in0=Odsa[:, :, 64], scalar1=1e-30, scalar2=None, op0=ALU.max), reads=["Odsa"], writes=["rdb2"])
                P.op("dve", lambda e: e.reciprocal(out=rdb[:, 8:13], in_=rdb[:, 8:13]), reads=["rdb2"], writes=["rdb2"])
                P.op("dve", lambda e: e.tensor_tensor(out=mixb[:, 320:640].rearrange("p (h d) -> p h d", d=64), in0=Odsa[:, :, 0:64], in1=rdb[:, 8:13].unsqueeze(2).to_broadcast([128, 5, 64]), op=ALU.mult),
                     reads=["Odsa", "rdb2"], writes=["mixb"])
                P.op("dve", lambda e: e.tensor_tensor(out=rdb[:, 13:15], in0=Odil[:, 0, :, 64], in1=Odil[:, 1, :, 64], op=ALU.add), reads=["Odil0", "Odil1"], writes=["rdb3"])
                P.op("dve", lambda e: e.tensor_tensor(out=rdb[:, 13:15], in0=rdb[:, 13:15], in1=Odil[:, 2, :, 64], op=ALU.add), reads=["rdb3", "Odil2"], writes=["rdb3"])
                P.op("dve", lambda e: e.reciprocal(out=rdb[:, 13:15], in_=rdb[:, 13:15]), reads=["rdb3"], writes=["rdb3"])
                for g in range(3):
                    P.op("dve", lambda e, g=g: e.tensor_tensor(out=mixb[:, 640 + g * 128:640 + (g + 1) * 128].rearrange("p (h d) -> p h d", d=64), in0=Odil[:, g, :, 0:64],
                                                               in1=rdb[:, 13:15].unsqueeze(2).to_broadcast([128, 2, 64]), op=ALU.mult), reads=["Odil%d" % g, "rdb3"], writes=["mixb"])
                if dbg and li == 0:
                    P.op("dve", lambda e: e.tensor_copy(out=score[:, 0:DM], in_=mixb[:]), reads=["mixb"], writes=["score"])
                    P.dma("sp", lambda e: e.dma_start(out=dbg_out["d_mix"][T0:T0 + 128, :], in_=score[:, 0:DM]), reads=["score"], writes=["d_mix"])
                for c in range(8):
                    P.op("pe", lambda e, c=c: e.transpose(out=psT[:, c * 128:(c + 1) * 128], in_=mixb[:, c * 128:(c + 1) * 128], identity=ident[:]), reads=["mixb", "ident"], writes=["psT"])
                P.op("act", lambda e: e.copy(out=mixT[:].rearrange("p c t -> p (c t)"), in_=psT[:, :]), reads=["psT"], writes=["hT"])
                for hf in range(2):
                    for c in range(8):
                        P.op("pe", lambda e, c=c, hf=hf: e.matmul(psA[:, hf * 512:(hf + 1) * 512], lhsT=mixT[:, c, :], rhs=w_out_sb[:, c, hf * 512:(hf + 1) * 512], start=(c == 0), stop=(c == 7)),
                             reads=["hT", "w_out_sb"], writes=["psA"])
                P.op("dve", lambda e, xb_=xb_: e.tensor_tensor(out=x1t[:], in0=psA[:, :], in1=xb_[:], op=ALU.add), reads=["psA", xk], writes=["xt"])
                P.dma("sp", lambda e: e.dma_start(out=xs1[T0:T0 + 128, :], in_=x1t[:]), reads=["xt"], writes=["xs1:%d" % n])
                if dbg and li == 0:
                    P.dma("sp", lambda e: e.dma_start(out=dbg_out["d_x1"][T0:T0 + 128, :], in_=x1t[:]), reads=["xt"], writes=["d_x1"])

            for n in range(NTT):
                tile_body(n)

            P.stage(14)
            P.barrier()
            for c in range(8):
                P.dma("pool", lambda e, c=c: e.dma_start(out=w_up_sb[:, c, :], in_=w_up[li, c * 128:(c + 1) * 128, :]), writes=["w_up_sb"])
            for c in range(NFC):
                P.dma("pool", lambda e, c=c: e.dma_start(out=w_dn_sb[:, c, :], in_=w_down[li, c * 128:(c + 1) * 128, :]), writes=["w_dn_sb"])
            P.dma("sp", lambda e: e.dma_start(out=cwst[:, 0:3, :], in_=w_cw[li].rearrange("k (c p) -> c k p", p=128)), writes=["cwst"])
            P.dma("sp", lambda e: e.dma_start(out=cwst[:, 3, :], in_=w_cb[li].rearrange("(c p) -> c p", p=128)), writes=["cwst"])
            for k in range(4):
                P.op("pe", lambda e, k=k: e.transpose(out=psP[:, k * 32:k * 32 + NFC], in_=cwst[:, k, :], identity=identf[0:NFC, 0:NFC]), reads=["cwst", "identf"], writes=["psP"])
            P.op("dve", lambda e: e.tensor_copy(out=cwall[:], in_=psP[:, 0:128].rearrange("p (k c) -> p k c", k=4)[:, :, 0:NFC]), reads=["psP"], writes=["cwall"])
            P.op("pool", lambda e: e.memset(carry[:], 0.0), writes=["carry"])
            if is_last and final:
                P.dma("sp", lambda e: e.dma_start(out=gfbc, in_=w_fin.partition_broadcast(128)), writes=["gfbc"])
            P.stage(15)
            NG = (NTT * 128 + TF - 1) // TF
            def ffn_group(gi):
                G0 = gi * TF
                nsub = min(TF // 128, NTT - gi * (TF // 128))
                TW = nsub * 128
                for sb_ in range(nsub):
                    P.dma("sp", lambda e, sb_=sb_: e.dma_start(out=xf[:, sb_, :], in_=xs1[G0 + sb_ * 128:G0 + (sb_ + 1) * 128, :]), reads=["xs1:%d" % (gi * (TF // 128) + sb_)], writes=["xf%d" % sb_])
                    P.op("dve", lambda e, sb_=sb_: e.scalar_tensor_tensor(out=junkb[:], in0=xf[:, sb_, :], scalar=1.0, in1=xf[:, sb_, :], op0=ALU.mult, op1=ALU.mult, accum_out=ss[:, 0:1]),
                         reads=["xf%d" % sb_], writes=["hb", "ss0"])
                    P.op("dve", lambda e: e.tensor_scalar(out=ss[:, 1:2], in0=ss[:, 0:1], scalar1=1.0 / DM, scalar2=EPS, op0=ALU.mult, op1=ALU.add), reads=["ss0"], writes=["ss1"])
                    P.op("pool!", lambda e: e.tensor_tensor(out=ss[:, 2:3], in0=ss[:, 1:2], in1=neghalf[:], op=ALU.pow), reads=["ss1", "neghalf"], writes=["ss2"])
                    P.op("dve", lambda e, sb_=sb_: e.tensor_scalar(out=hb[:], in0=xf[:, sb_, :], scalar1=ss[:, 2:3], scalar2=None, op0=ALU.mult),
                         reads=["xf%d" % sb_, "ss2"], writes=["hb"])
                    for c in range(8):
                        P.op("pe", lambda e, c=c: e.transpose(out=psT[:, c * 128:(c + 1) * 128], in_=hb[:, c * 128:(c + 1) * 128], identity=ident[:]), reads=["hb", "ident"], writes=["psT"])
                    P.op("dve", lambda e, sb_=sb_: e.tensor_tensor(out=h2T[:, :, sb_ * 128:(sb_ + 1) * 128], in0=psT[:, :].rearrange("p (c t) -> p c t", c=8),
                                                                 in1=gcol[:, 8:16].unsqueeze(2).to_broadcast([128, 8, 128]), op=ALU.mult), reads=["psT", "gcol"], writes=["h2T"])
                for fc in range(NFC):
                    pa = psA[:, 0:TW]
                    pu = psA[:, 512:512 + TW]
                    if fc % 2 == 1:
                        pa = psB[:, 0:TW]
                        pu = psB[:, 512:512 + TW]
                    pk = "psA" if fc % 2 == 0 else "psB"
                    for c in range(8):
                        P.op("pe", lambda e, c=c, fc=fc, pa=pa: e.matmul(pa, lhsT=w_up_sb[:, c, fc * 128:(fc + 1) * 128], rhs=h2T[:, c, 0:TW], start=(c == 0), stop=(c == 7)),
                             reads=["w_up_sb", "h2T"], writes=[pk])
                    for c in range(8):
                        P.op("pe", lambda e, c=c, fc=fc, pu=pu: e.matmul(pu, lhsT=w_up_sb[:, c, DFF + fc * 128:DFF + (fc + 1) * 128], rhs=h2T[:, c, 0:TW], start=(c == 0), stop=(c == 7)),
                             reads=["w_up_sb", "h2T"], writes=[pk])
                    P.op("pool", lambda e, fc=fc: e.tensor_copy(out=abuf[:, 0:2], in_=carry[:, fc, :]), reads=["carry"], writes=["abuf"])
                    P.op("act", lambda e, pa=pa: e.copy(out=abuf[:, 2:TW + 2], in_=pa), reads=[pk], writes=["abuf"])
                    P.op("pool", lambda e, fc=fc: e.tensor_copy(out=carry[:, fc, :], in_=abuf[:, TW:TW + 2]), reads=["abuf"], writes=["carry"])
                    P.op("dve", lambda e, fc=fc: e.tensor_scalar(out=accb[:, 0:TW], in0=abuf[:, 2:TW + 2], scalar1=cwall[:, 2, fc:fc + 1], scalar2=cwall[:, 3, fc:fc + 1], op0=ALU.mult, op1=ALU.add),
                         reads=["abuf", "cwall"], writes=["accb"])
                    P.op("dve", lambda e, fc=fc: e.scalar_tensor_tensor(out=accb[:, 0:TW], in0=abuf[:, 1:TW + 1], scalar=cwall[:, 1, fc:fc + 1], in1=accb[:, 0:TW], op0=ALU.mult, op1=ALU.add),
                         reads=["abuf", "cwall", "accb"], writes=["accb"])
                    P.op("dve", lambda e, fc=fc: e.scalar_tensor_tensor(out=accb[:, 0:TW], in0=abuf[:, 0:TW], scalar=cwall[:, 0, fc:fc + 1], in1=accb[:, 0:TW], op0=ALU.mult, op1=ALU.add),
                         reads=["abuf", "cwall", "accb"], writes=["accb"])
                    P.op("act", lambda e: e.activation(out=silb[:, 0:TW], in_=accb[:, 0:TW], func=AF.Silu), reads=["accb"], writes=["silb"])
                    P.op("dve", lambda e, fc=fc, pu=pu: e.tensor_tensor(out=gT[:, fc, 0:TW], in0=pu, in1=silb[:, 0:TW], op=ALU.mult), reads=[pk, "silb"], writes=["gT"])
                for sb_ in range(nsub):
                    for hf in range(2):
                        for fc in range(NFC):
                            P.op("pe", lambda e, fc=fc, hf=hf, sb_=sb_: e.matmul(psO[:, hf * 512:(hf + 1) * 512], lhsT=gT[:, fc, sb_ * 128:(sb_ + 1) * 128], rhs=w_dn_sb[:, fc, hf * 512:(hf + 1) * 512],
                                                                                 start=(fc == 0), stop=(fc == NFC - 1)), reads=["gT", "w_dn_sb"], writes=["psO%d" % hf])
                    P.op("dve", lambda e, sb_=sb_: e.tensor_tensor(out=x2t[:], in0=psO[:, :], in1=xf[:, sb_, :], op=ALU.add), reads=["psO0", "psO1", "xf%d" % sb_], writes=["x2t"])
                    r0 = G0 + sb_ * 128
                    if is_last and final:
                        P.op("dve", lambda e: e.scalar_tensor_tensor(out=junkb[:], in0=x2t[:], scalar=1.0, in1=x2t[:], op0=ALU.mult, op1=ALU.mult, accum_out=ss[:, 0:1]), reads=["x2t"], writes=["hb", "ss0"])
                        P.op("dve", lambda e: e.tensor_scalar(out=ss[:, 1:2], in0=ss[:, 0:1], scalar1=1.0 / DM, scalar2=EPS, op0=ALU.mult, op1=ALU.add), reads=["ss0"], writes=["ss1"])
                        P.op("pool!", lambda e: e.tensor_tensor(out=ss[:, 2:3], in0=ss[:, 1:2], in1=neghalf[:], op=ALU.pow), reads=["ss1", "neghalf"], writes=["ss2"])
                        P.op("dve", lambda e: e.scalar_tensor_tensor(out=x2t[:], in0=x2t[:], scalar=ss[:, 2:3], in1=gfbc[:], op0=ALU.mult, op1=ALU.mult), reads=["x2t", "ss2", "gfbc"], writes=["x2t"])
                    P.dma("sp", lambda e, r0=r0: e.dma_start(out=xout_ap[r0:r0 + 128, :], in_=x2t[:]), reads=["x2t"], writes=["xout:%d" % (r0 // 128)])

            for gi in range(NG):
                ffn_group(gi)

        cur_in = x_in
        for li in range(n_layers):
            is_last = li == n_layers - 1
            xout = y_out if is_last else xs2
            emit_layer(li, cur_in, xout, is_last)
            cur_in = xs2
        P.final_wait("sp", ["xout:%d" % i for i in range(NTT)] + (["d_pf", "d_mix", "d_x1"] if dbg else []))
        P.enabled = True
        P.barrier()
        print("ops recorded:", P.nops, "sbuf remaining:", nc.sbuf_bytes_remaining, "att_end", att_end, "ffn_end", ffn_end)
        P.emit(st)
    return nc


_CACHE = {}


def _get_prog(n_layers, final):
    key = (n_layers, final)
    if key not in _CACHE:
        _CACHE[key] = build(n_layers, final)
    return _CACHE[key]


FUSED = True


def kernel(x, norm1_g, w_in, cmp_pos, cmp_w1, cmp_w2, w_out, norm2_g, w_up, conv_w, conv_b, w_down, final_g):
    f = lambda a: np.ascontiguousarray(np.asarray(a, dtype=np.float32))
    x = f(x)
    W = dict(norm1_g=f(norm1_g), w_in=f(w_in), cmp_pos=f(cmp_pos), cmp_w1=f(cmp_w1), cmp_w2=f(cmp_w2), w_out=f(w_out),
             norm2_g=f(norm2_g), w_up=f(w_up), conv_w=f(conv_w), conv_b=f(conv_b), w_down=f(w_down))
    consts = _consts()
    B = x.shape[0]
    depth = W["w_in"].shape[0]
    cur = x
    if FUSED:
        nc = _get_prog(depth, True)
        in_maps = []
        for c in range(8):
            m = dict(x=cur[c % B], final_g=f(final_g))
            m.update(W)
            m.update(consts)
            in_maps.append(m)
        res = run_bass_kernel_spmd(nc, in_maps, core_ids=list(range(8)))
        return np.stack([res.results[b]["y"] for b in range(B)], axis=0)
    for li in range(depth):
        last = li == depth - 1
        nc = _get_prog(1, last)
        in_maps = []
        for c in range(8):
            m = dict(x=np.ascontiguousarray(cur[c % B]), final_g=f(final_g))
            m.update({k: np.ascontiguousarray(v[li:li + 1]) for k, v in W.items()})
            m.update(consts)
            in_maps.append(m)
        res = run_bass_kernel_spmd(nc, in_maps, core_ids=list(range(8)))
        cur = np.stack([res.results[b]["y"] for b in range(B)], axis=0)
    return cur
```

```python
import math
from contextlib import ExitStack
import numpy as np
import concourse.bass as bass
import concourse.mybir as mybir
from concourse.bass_utils import run_bass_kernel_spmd

F32 = mybir.dt.float32
BF16 = mybir.dt.bfloat16
ALU = mybir.AluOpType
AF = mybir.ActivationFunctionType
AX = mybir.AxisListType

L = 4096
NT = L // 128
DM = 1024
DIN = 2615
DFF = 2816
NFC = DFF // 128
EPS = 1e-6
MASKV = -30000.0
NEGBIG = -1e30
SCALE = 0.125
DIL = ((128, 1), (512, 4), (2048, 16))
DIL_O = (1, 4, 16)
DIL_R = (2, 5, 17)
O_AQ, O_AKC, O_AVC, O_AKS, O_AVS, O_AKW, O_AVW, O_AG = 0, 320, 384, 448, 512, 576, 640, 704
O_BQ, O_BK, O_BV, O_BIQ, O_BIK, O_BIW = 719, 1039, 1103, 1167, 1423, 1455
O_CQ, O_CK, O_CV = 1463, 1847, 2231
BIS_ITERS = 20
BIS_LO = -16.0

ENGS = ("pe", "act", "dve", "pool", "sp")
POOL_TO_DVE = False


class Prog:
    def __init__(self, nc, n_dma_slots=4):
        self.nc = nc
        self.streams = {e: [] for e in ENGS}
        self.cnt = {e: 0 for e in ENGS}
        self.sems = {}
        self.waited = {e: {} for e in ENGS}
        self.last_w = {}
        self.readers = {}
        self.n_dma_slots = n_dma_slots
        self.dma_rr = {e: 0 for e in ENGS}
        self.nops = 0
        self.enabled = True
        self.limit = None

    def stage(self, k):
        if self.limit is not None and k > self.limit:
            self.enabled = False

    def _deps(self, eng, reads, writes):
        deps = {}

        def add(src, val):
            if src == "pe" and eng == "pe":
                return
            if val > deps.get(src, 0):
                deps[src] = val
        for k in reads:
            lw = self.last_w.get(k)
            if lw:
                add(*lw)
        for k in writes:
            lw = self.last_w.get(k)
            if lw:
                add(*lw)
            for r in self.readers.get(k, ()):
                add(*r)
        return deps

    def _emit_waits(self, eng, deps):
        waits = []
        for src, val in deps.items():
            if val > self.waited[eng].get(src, 0):
                self.waited[eng][src] = val
                waits.append((src, val))
        return waits

    def _commit(self, src, val, reads, writes):
        for k in reads:
            lst = self.readers.setdefault(k, [])
            lst[:] = [r for r in lst if r[0] != src]
            lst.append((src, val))
        for k in writes:
            self.last_w[k] = (src, val)
            self.readers[k] = []

    def op(self, eng, fn, reads=(), writes=()):
        if not self.enabled:
            return
        if eng == "pool!":
            eng = "pool"
        elif eng == "pool" and POOL_TO_DVE:
            eng = "dve"
        psr = [k for k in reads if k.startswith("ps")]
        if psr:
            writes = list(writes) + psr
            reads = [k for k in reads if not k.startswith("ps")]
        deps = self._deps(eng, reads, writes)
        waits = self._emit_waits(eng, deps)
        self.cnt[eng] += 1
        self.streams[eng].append((waits, fn, (eng, 1)))
        self._commit(eng, self.cnt[eng], reads, writes)
        self.nops += 1

    def dma(self, eng, fn, reads=(), writes=()):
        if not self.enabled:
            return
        slot = self.dma_rr[eng]
        self.dma_rr[eng] = (slot + 1) % self.n_dma_slots
        src = ("dma", eng, slot)
        if src not in self.cnt:
            self.cnt[src] = 0
        deps = self._deps(eng, reads, writes)
        if self.cnt[src] > 0:
            deps[src] = max(deps.get(src, 0), self.cnt[src])
        waits = self._emit_waits(eng, deps)
        self.cnt[src] += 16
        self.streams[eng].append((waits, fn, (src, 16)))
        self._commit(src, self.cnt[src], reads, writes)
        self.nops += 1

    def barrier(self):
        snap = dict(self.cnt)
        for e in ENGS:
            deps = {s: v for s, v in snap.items() if v > 0 and not (s == "pe" and e == "pe")}
            waits = self._emit_waits(e, deps)
            if waits:
                self.streams[e].append((waits, None, None))

    def final_wait(self, eng, keys):
        deps = {}
        for k in keys:
            lw = self.last_w.get(k)
            if lw and lw[1] > deps.get(lw[0], 0):
                deps[lw[0]] = lw[1]
        waits = self._emit_waits(eng, deps)
        self.streams[eng].append((waits, None, None))

    def emit(self, stack):
        nc = self.nc
        for src in self.cnt:
            name = "s_" + (src if isinstance(src, str) else "_".join(str(x) for x in src))
            self.sems[src] = stack.enter_context(nc.semaphore(name))
        targets = {e: set() for e in ENGS}
        for e in ENGS:
            for waits, fn, inc in self.streams[e]:
                for src, val in waits:
                    if isinstance(src, str):
                        targets[src].add(val)
        remap = {e: {v: i + 1 for i, v in enumerate(sorted(targets[e]))} for e in ENGS}
        block = stack.enter_context(nc.Block())
        reg = {"pe": block.tensor, "act": block.scalar, "dve": block.vector, "pool": block.gpsimd, "sp": block.sync}
        for e in ENGS:
            stream = self.streams[e]
            if not stream:
                continue

            def body(engine, stream=stream, e=e):
                k = 0
                for waits, fn, inc in stream:
                    for src, val in waits:
                        if isinstance(src, str):
                            engine.wait_ge(self.sems[src], remap[src][val])
                        else:
                            engine.wait_ge(self.sems[src], val)
                    if fn is not None:
                        ins = fn(engine)
                        if isinstance(inc[0], str):
                            k += 1
                            if k in remap[e]:
                                ins.then_inc(self.sems[e], 1)
                        else:
                            ins.then_inc(self.sems[inc[0]], inc[1])
            reg[e](body)


def _consts():
    t = np.arange(128)[:, None]
    s = np.arange(128)[None, :]
    tiles = []
    tiles.append(np.where(s <= t, 0.0, MASKV))
    tiles.append(np.where(s > t, 0.0, MASKV))
    for (w, r), og in zip(DIL, DIL_O):
        for o in range(og + 1):
            d = 128 * o + t - s
            ok = (d >= 0) & (d % r == 0) & (d <= 128 * r)
            tiles.append(np.where(ok, 0.0, MASKV))
    tiles.append(np.where(s <= t, 0.0, NEGBIG))
    bias = np.stack(tiles, axis=1).astype(np.float32)
    ident = np.eye(128, dtype=np.float32)
    i4 = np.concatenate([ident] * 4, axis=1)
    def tab(rot):
        inv = 1.0 / (500000.0 ** (np.arange(0, rot, 2, dtype=np.float32) / np.float32(rot)))
        ang = np.arange(L, dtype=np.float32)[:, None] * inv[None, :]
        cs = np.concatenate([np.cos(ang), np.sin(ang)], axis=1).astype(np.float32)
        return np.ascontiguousarray(cs.reshape(NT, 128, rot).transpose(1, 0, 2))
    cs_h = tab(16)
    cs_i = tab(8)
    n = np.arange(256)[:, None]
    j = np.arange(64)[None, :]
    ov = ((n >= 4 * j - 1) & (n <= 4 * j + 3) & (n <= 254)).astype(np.float32)
    ov = np.ascontiguousarray(ov.reshape(2, 128, 64).transpose(1, 0, 2))
    cm = []
    for (n_, nt_) in _cmp_pairs():
        base = 128 * n_ - 2048 * nt_ - 31
        cm.append(np.where(base + t - 16 * s >= 0, 0.0, MASKV))
    cmpb = np.stack(cm, axis=0).astype(np.float32)
    return dict(c_bias=bias, c_ident=ident, c_i4=i4, c_csh=cs_h, c_csi=cs_i, c_ov=ov, c_cmpb=cmpb)


def _cmp_pairs():
    out = []
    for n_ in range(NT):
        for nt_ in range(2):
            base = 128 * n_ - 2048 * nt_ - 31
            if base + 127 < 0:
                continue
            if base < 16 * 127:
                out.append((n_, nt_))
    return out


NB_BIAS = 2 + sum(o + 1 for o in DIL_O) + 1


def _dil_bias_idx(g, o):
    return 2 + sum(DIL_O[k] + 1 for k in range(g)) + o


def build(n_layers, final, dbg=False, nt_limit=None, stage_limit=None):
    nc = bass.Bass("TRN2", target_bir_lowering=False)
    NTT = NT if nt_limit is None else nt_limit

    def din(name, shape):
        return nc.dram_tensor(name, list(shape), F32, kind="ExternalInput").ap()

    x_in = din("x", [L, DM])
    w_norm1 = din("norm1_g", [n_layers, DM])
    w_in = din("w_in", [n_layers, DM, DIN])
    w_pos = din("cmp_pos", [n_layers, 2, 32, 64])
    w_c1 = din("cmp_w1", [n_layers, 2, 2048, 128])
    w_c2 = din("cmp_w2", [n_layers, 2, 128, 64])
    w_outd = din("w_out", [n_layers, DM, DM])
    w_norm2 = din("norm2_g", [n_layers, DM])
    w_up = din("w_up", [n_layers, DM, 2 * DFF])
    w_cw = din("conv_w", [n_layers, 3, DFF])
    w_cb = din("conv_b", [n_layers, DFF])
    w_down = din("w_down", [n_layers, DFF, DM])
    w_fin = din("final_g", [DM])
    c_bias = din("c_bias", [128, NB_BIAS, 128])
    c_ident = din("c_ident", [128, 128])
    c_i4 = din("c_i4", [128, 512])
    c_csh = din("c_csh", [128, NT, 16])
    c_csi = din("c_csi", [128, NT, 8])
    c_ov = din("c_ov", [128, 2, 64])
    cmp_pairs = _cmp_pairs()
    c_cmpb = din("c_cmpb", [len(cmp_pairs), 128, 128])
    y_out = nc.dram_tensor("y", [L, DM], F32, kind="ExternalOutput").ap()
    xs1 = nc.dram_tensor("xs1", [L, DM], F32, kind="Internal").ap()
    xs2 = nc.dram_tensor("xs2", [L, DM], F32, kind="Internal").ap()
    dbg_out = {}
    if dbg:
        dbg_out["d_pf"] = nc.dram_tensor("d_pf", [L, DIN], F32, kind="ExternalOutput").ap()
        dbg_out["d_mix"] = nc.dram_tensor("d_mix", [L, DM], F32, kind="ExternalOutput").ap()
        dbg_out["d_x1"] = nc.dram_tensor("d_x1", [L, DM], F32, kind="ExternalOutput").ap()

    st = ExitStack()
    with st:
        def SB(name, shape, dt):
            return st.enter_context(nc.sbuf_tensor(name, list(shape), dt))

        def PS(name, shape, dt):
            return st.enter_context(nc.psum_tensor(name, list(shape), dt))

        P = Prog(nc)
        P.limit = stage_limit

        psA = PS("psA", [128, 1024], F32)
        psB = PS("psB", [128, 1024], F32)
        psO = PS("psO", [128, 1024], F32)
        psP = PS("psP", [128, 512], F32)
        psT = PS("psT", [128, 1024], BF16)

        ident = SB("ident", [128, 128], BF16)
        i4 = SB("i4", [128, 512], BF16)
        biasT = SB("biasT", [128, NB_BIAS, 128], BF16)
        csh = SB("csh", [128, NT, 16], F32)
        csi = SB("csi", [128, NT, 8], F32)
        neghalf = SB("neghalf", [128, 1], F32)
        zerosb = SB("zerosb", [128, 128], BF16)

        UCOLS = 75520
        U = SB("U", [128, UCOLS], BF16)
        U2COLS = 7800
        U2 = SB("U2", [128, U2COLS], F32)
        ucur = [0]
        u2cur = [0]

        def carve(ncols):
            a = ucur[0]
            ucur[0] += ncols
            assert ucur[0] <= UCOLS, ucur[0]
            return U[:, a:a + ncols]

        def carve2(ncols):
            a = u2cur[0]
            u2cur[0] += ncols
            assert u2cur[0] <= U2COLS, u2cur[0]
            return U2[:, a:a + ncols]

        w_in_sb = carve(8 * DIN).rearrange("p (c n) -> p c n", c=8)
        w_out_sb = carve(8 * DM).rearrange("p (c n) -> p c n", c=8)
        KT_a = carve(L)
        KT_b = carve(L)
        kiT4 = carve(L)
        kcvT = carve(L)
        KT_c = [carve(DIL_R[g] * 128).rearrange("p (r t) -> p r t", t=128) for g in range(3)]
        Vs_a = carve(NT * 65).rearrange("p (n w) -> p n w", w=65)
        Vw_a = carve(5 * 65).rearrange("p (n w) -> p n w", w=65)
        V_b = carve(NT * 65).rearrange("p (n w) -> p n w", w=65)
        V_c = [carve(DIL_R[g] * 130).rearrange("p (r h w) -> p r h w", h=2, w=65) for g in range(3)]
        mb_d = carve(L)
        mb_s = mb_d
        Rbuf = carve(4 * 512).rearrange("p (h w) -> p h w", h=4)
        W1_sb = carve(32 * 128).rearrange("p (j h) -> p j h", j=32)
        TB_AQ, TB_AQR, TB_KA, TB_KCV, TB_B, TB_QI, TB_KI, TB_C = 0, 320, 960, 1088, 1216, 1600, 1856, 1984
        TBW = 1984 + 768
        Tb = carve(TBW)
        Dh = carve(8 * 128).rearrange("p (h t) -> p h t", h=8)
        QT_araw = carve(640)
        QT_ar = carve(640)
        QT_b = carve(640)
        qiT = carve(256).rearrange("p (a t) -> p a t", a=2)
        QT_c = carve(384).rearrange("p (a t) -> p a t", a=3)
        PT = [carve(640), carve(640)]
        mixb = carve(DM)
        att_end = ucur[0]
        ucur[0] = 0
        TF = 256
        w_up_sb = carve(8 * 2 * DFF).rearrange("p (c n) -> p c n", c=8)
        w_dn_sb = carve(NFC * DM).rearrange("p (c n) -> p c n", c=NFC)
        h2T = carve(8 * TF).rearrange("p (c t) -> p c t", c=8)
        gT = carve(NFC * TF).rearrange("p (c t) -> p c t", c=NFC)
        silb = carve(TF)
        ffn_end = ucur[0]
        score = carve2(L)
        Pf = carve2(DIN)
        xt1 = carve2(DM)
        xt = [xt1, xt1]
        x1t = xt1
        Ocmp = score[:, 0:645].rearrange("p (h w) -> p h w", h=5)
        Oslc = score[:, 645:970].rearrange("p (h w) -> p h w", h=5)
        Odsa = score[:, 1295:1620].rearrange("p (h w) -> p h w", h=5)
        tmpo = score[:, 2010:2650].rearrange("p (a h d) -> p a h d", a=2, h=5)
        att2_end = u2cur[0]
        u2cur[0] = 0
        xf = carve2(2 * DM).rearrange("p (a n) -> p a n", a=2)
        x2t = carve2(DM)
        abuf = carve2(TF + 2)
        accb = carve2(TF)
        gfbc = carve2(DM)
        cwst = carve2(4 * 128)[0:NFC, :].rearrange("p (k t) -> p k t", k=4)

        Owin = SB("Owin", [128, 5, 65], F32)
        Odil = SB("Odil", [128, 3, 2, 65], F32)
        gcol = SB("gcol", [128, 16], F32)
        gst = SB("gst", [8, 2, 128], F32)
        hb = SB("hb", [128, DM], BF16)
        junkb = hb
        hT = SB("hT", [128, 8, 128], BF16)
        mixT = hT
        ss = SB("ss", [128, 4], F32)
        W2_sb = SB("W2_sb", [128, 2, 64], BF16)
        posT = SB("posT", [128, 32], BF16)
        constc = SB("constc", [128, 2], F32)
        kcmpT = SB("kcmpT", [64, 256], BF16)
        Vc = SB("Vc", [128, 2, 129], BF16)
        zs = SB("zs", [128, 5, 32], F32)
        hidT = SB("hidT", [128, 32], BF16)
        vst = SB("vst", [8, 64], BF16)
        ropet = SB("ropet", [128, 4, 12, 8], F32)
        iqf = SB("iqf", [128, 9, 32], F32)
        iws = SB("iws", [128, 3, 8], F32)
        gs = SB("gs", [128, 15], F32)
        dens = SB("dens", [128, 15], F32)
        coef = SB("coef", [128, 15], F32)
        rdb = SB("rdb", [128, 16], F32)
        impf = SB("impf", [128, 2, 64], F32)
        fb = SB("fb", [128, 64], F32)
        m8 = SB("m8", [128, 16], F32)
        bmb = SB("bmb", [128, 64], BF16)
        bis = SB("bis", [128, 4], F32)
        carry = SB("carry", [128, NFC, 2], F32)
        cwall = SB("cwall", [128, 4, NFC], F32)
        identf = SB("identf", [128, 128], F32)

        P.dma("pool", lambda e: e.dma_start(out=ident[:], in_=c_ident[:, :]), writes=["ident"])
        P.dma("pool", lambda e: e.dma_start(out=i4[:], in_=c_i4[:, :]), writes=["i4"])
        P.dma("pool", lambda e: e.dma_start(out=biasT[:], in_=c_bias[:, :, :]), writes=["biasT"])
        P.dma("sp", lambda e: e.dma_start(out=csh[:], in_=c_csh[:, :, :]), writes=["csh"])
        P.dma("sp", lambda e: e.dma_start(out=csi[:], in_=c_csi[:, :, :]), writes=["csi"])
        P.dma("sp", lambda e: e.dma_start(out=identf[:], in_=c_ident[:, :]), writes=["identf"])
        P.op("pool", lambda e: e.memset(neghalf[:], -0.5), writes=["neghalf"])
        P.op("pool", lambda e: e.memset(zerosb[:], 0.0), writes=["zerosb"])

        sig = lambda e, out, in_: None

        def emit_layer(li, xin_ap, xout_ap, is_last):
            P.stage(1)
            P.barrier()
            for c in range(8):
                P.dma("pool", lambda e, c=c: e.dma_start(out=w_in_sb[:, c, :], in_=w_in[li, c * 128:(c + 1) * 128, :]), writes=["w_in_sb"])
            for c in range(8):
                P.dma("pool", lambda e, c=c: e.dma_start(out=w_out_sb[:, c, :], in_=w_outd[li, c * 128:(c + 1) * 128, :]), writes=["w_out_sb"])
            P.dma("sp", lambda e: e.dma_start(out=gst[:, 0, :], in_=w_norm1[li].rearrange("(c p) -> c p", p=128)), writes=["gst"])
            P.dma("sp", lambda e: e.dma_start(out=gst[:, 1, :], in_=w_norm2[li].rearrange("(c p) -> c p", p=128)), writes=["gst"])
            for k in range(2):
                P.op("pe", lambda e, k=k: e.transpose(out=psP[:, k * 8:(k + 1) * 8], in_=gst[:, k, :], identity=identf[0:8, 0:8]), reads=["gst", "identf"], writes=["psP"])
            P.op("dve", lambda e: e.tensor_copy(out=gcol[:], in_=psP[:, 0:16]), reads=["psP"], writes=["gcol"])
            for c in range(2):
                P.dma("pool", lambda e, c=c: e.dma_start(out=W1_sb[c * 64:(c + 1) * 64, :, :], in_=w_c1[li, c].rearrange("(j d) h -> d j h", d=64)), writes=["W1_sb"])
                P.dma("pool", lambda e, c=c: e.dma_start(out=W2_sb[:, c, :], in_=w_c2[li, c]), writes=["W2_sb"])
                P.dma("pool", lambda e, c=c: e.dma_start(out=posT[c * 64:(c + 1) * 64, :], in_=w_pos[li, c].rearrange("j d -> d j"), allow_slow_non_contiguous=True), writes=["posT"])
            P.dma("pool", lambda e: e.dma_start(out=Vc[:, :, 65:129], in_=c_ov[:, :, :]), writes=["Vc"])
            P.op("pool", lambda e: e.memset(Vc[:, :, 0:64], 0.0), writes=["Vc"])
            P.op("pool", lambda e: e.memset(Vc[:, :, 64:65], 1.0), writes=["Vc"])
            P.op("pool", lambda e: e.memset(kcmpT[:], 0.0), writes=["kcmpT"])
            P.op("pool", lambda e: e.memset(Vs_a[:, :, 64:65], 1.0), writes=["Vs_a_ones"])
            P.op("pool", lambda e: e.memset(Vw_a[:, :, 64:65], 1.0), writes=["Vw_a_ones"])
            P.op("pool", lambda e: e.memset(V_b[:, :, 64:65], 1.0), writes=["V_b_ones"])
            for g in range(3):
                P.op("pool", lambda e, g=g: e.memset(V_c[g][:, :, :, 64:65], 1.0), writes=["V_c_ones%d" % g])
            for c in range(2):
                for jj in range(32):
                    P.op("pe", lambda e, c=c, jj=jj: e.matmul((psP if c == 0 else psA)[:, 0:1], lhsT=W1_sb[c * 64:(c + 1) * 64, jj, :], rhs=posT[c * 64:(c + 1) * 64, jj:jj + 1],
                                                             start=(jj == 0), stop=(jj == 31), skip_group_check=True),
                         reads=["W1_sb", "posT"], writes=["psP" if c == 0 else "psA"])
            P.op("dve", lambda e: e.tensor_copy(out=constc[:, 0:1], in_=psP[:, 0:1]), reads=["psP"], writes=["constc"])
            P.op("dve", lambda e: e.tensor_copy(out=constc[:, 1:2], in_=psA[:, 0:1]), reads=["psA"], writes=["constc"])

            st_toggle = [0]

            def attention(name, H, W, items, qfn, Ops, Okeys, Osb, Osb_key):
                nitems = len(items)
                first_in_bank = {}

                def one_item(ii, ktf, vf, bias_ap, rkeys, shared):
                    b = st_toggle[0]
                    st_toggle[0] ^= 1
                    psX = psA if b == 0 else psB
                    pskey = "psA" if b == 0 else "psB"
                    ptb = PT[b]
                    ptkey = "PT%d" % b
                    nob = bias_ap is None
                    if shared:
                        h1 = min(H, 4)
                        P.op("pe", lambda e: e.matmul(psX[:, 0:h1 * 128], lhsT=ktf(0), rhs=qfn(0, h1), start=True, stop=nob, skip_group_check=True),
                             reads=rkeys + [name + "_Q"], writes=[pskey])
                        if H > 4:
                            P.op("pe", lambda e: e.matmul(psX[:, 512:512 + (H - 4) * 128], lhsT=ktf(0), rhs=qfn(4, H), start=True, stop=nob, skip_group_check=True),
                                 reads=rkeys + [name + "_Q"], writes=[pskey])
                    else:
                        for h in range(H):
                            P.op("pe", lambda e, h=h: e.matmul(psX[:, h * 512:h * 512 + 128], lhsT=ktf(h), rhs=qfn(h, h + 1), start=True, stop=nob, skip_group_check=True),
                                 reads=rkeys + [name + "_Q"], writes=[pskey])
                    if not nob and not shared:
                        for h in range(H):
                            P.op("pe", lambda e, h=h: e.matmul(psX[:, h * 512:h * 512 + 128], lhsT=bias_ap, rhs=i4[:, 0:128], start=False, stop=True, skip_group_check=True),
                                 reads=rkeys + ["i4"], writes=[pskey])
                    if not nob and shared:
                        h1 = min(H, 4)
                        P.op("pe", lambda e: e.matmul(psX[:, 0:h1 * 128], lhsT=bias_ap, rhs=i4[:, 0:h1 * 128], start=False, stop=True, skip_group_check=True),
                             reads=rkeys + ["i4"], writes=[pskey])
                        if H > 4:
                            P.op("pe", lambda e: e.matmul(psX[:, 512:512 + (H - 4) * 128], lhsT=bias_ap, rhs=i4[:, 0:(H - 4) * 128], start=False, stop=True, skip_group_check=True),
                                 reads=rkeys + ["i4"], writes=[pskey])
                    if shared:
                        P.op("act", lambda e: e.activation(out=ptb[:, 0:H * 128], in_=psX[:, 0:H * 128], func=AF.Exp, scale=SCALE),
                             reads=[pskey], writes=[ptkey])
                    else:
                        P.op("act", lambda e: e.activation(out=ptb[:, 0:H * 128].rearrange("p (h t) -> p h t", h=H), in_=psX[:, :].rearrange("p (h c) -> p h c", h=2)[:, 0:H, 0:128], func=AF.Exp, scale=SCALE),
                             reads=[pskey], writes=[ptkey])
                    for h in range(H):
                        oap, okey = Ops(h)
                        fst = okey not in first_in_bank
                        first_in_bank[okey] = True
                        P.op("pe", lambda e, oap=oap, h=h, fst=fst: e.matmul(oap, lhsT=ptb[:, h * 128:(h + 1) * 128], rhs=vf(h), start=fst, stop=(ii == nitems - 1), skip_group_check=True),
                             reads=[ptkey] + rkeys, writes=[okey])

                for ii, (ktf, vf, bias_ap, rkeys, shared) in enumerate(items):
                    one_item(ii, ktf, vf, bias_ap, rkeys, shared)

            def tile_body(n):
                xb_ = xt[n % 2]
                xk = "xt"
                T0 = n * 128
                P.stage(2)
                P.dma("sp", lambda e, xb_=xb_: e.dma_start(out=xb_[:], in_=xin_ap[T0:T0 + 128, :]), writes=[xk])
                P.op("dve", lambda e, xb_=xb_: e.scalar_tensor_tensor(out=junkb[:], in0=xb_[:], scalar=1.0, in1=xb_[:], op0=ALU.mult, op1=ALU.mult, accum_out=ss[:, 0:1]),
                     reads=[xk], writes=["hb", "ss0"])
                P.op("dve", lambda e: e.tensor_scalar(out=ss[:, 1:2], in0=ss[:, 0:1], scalar1=1.0 / DM, scalar2=EPS, op0=ALU.mult, op1=ALU.add), reads=["ss0"], writes=["ss1"])
                P.op("pool!", lambda e: e.tensor_tensor(out=ss[:, 2:3], in0=ss[:, 1:2], in1=neghalf[:], op=ALU.pow), reads=["ss1", "neghalf"], writes=["ss2"])
                P.op("dve", lambda e, xb_=xb_: e.tensor_scalar(out=hb[:], in0=xb_[:], scalar1=ss[:, 2:3], scalar2=None, op0=ALU.mult),
                     reads=[xk, "ss2"], writes=["hb"])
                for c in range(8):
                    P.op("pe", lambda e, c=c: e.transpose(out=psT[:, c * 128:(c + 1) * 128], in_=hb[:, c * 128:(c + 1) * 128], identity=ident[:]), reads=["hb", "ident"], writes=["psT"])
                P.op("dve", lambda e: e.tensor_tensor(out=hT[:], in0=psT[:, :].rearrange("p (c t) -> p c t", c=8), in1=gcol[:, 0:8].unsqueeze(2).to_broadcast([128, 8, 128]), op=ALU.mult),
                     reads=["psT", "gcol"], writes=["hT"])
                P.stage(3)
                ngrp = (DIN + 511) // 512
                for gi in range(ngrp):
                    c0 = gi * 512
                    c1 = min(DIN, c0 + 512)
                    psX = psA if gi % 2 == 0 else psB
                    pk = "psA" if gi % 2 == 0 else "psB"
                    for c in range(8):
                        P.op("pe", lambda e, c=c, c0=c0, c1=c1, psX=psX: e.matmul(psX[:, 0:c1 - c0], lhsT=hT[:, c, :], rhs=w_in_sb[:, c, c0:c1], start=(c == 0), stop=(c == 7)),
                             reads=["hT", "w_in_sb"], writes=[pk])
                    if gi % 2 == 0:
                        P.op("act", lambda e, c0=c0, c1=c1, psX=psX: e.copy(out=Pf[:, c0:c1], in_=psX[:, 0:c1 - c0]), reads=[pk], writes=["Pf"])
                    else:
                        P.op("dve", lambda e, c0=c0, c1=c1, psX=psX: e.tensor_copy(out=Pf[:, c0:c1], in_=psX[:, 0:c1 - c0]), reads=[pk], writes=["Pf"])
                if dbg and li == 0:
                    P.dma("sp", lambda e: e.dma_start(out=dbg_out["d_pf"][T0:T0 + 128, :], in_=Pf[:]), reads=["Pf"], writes=["d_pf"])

                P.stage(4)
                def rope_set(src_ap, H, dst_ap, tab, half, D, dup=False, key="Tb"):
                    cosb = tab[:, n, 0:half].unsqueeze(1).to_broadcast([128, H, half])
                    sinb = tab[:, n, half:2 * half].unsqueeze(1).to_broadcast([128, H, half])
                    x1 = src_ap[:, :, 0:half]
                    x2 = src_ap[:, :, half:2 * half]
                    t = [ropet[:, k, 0:H, 0:half] for k in range(4)]
                    rk = ["Pf", "csh", "csi"]
                    P.op("dve", lambda e: e.tensor_tensor(out=t[0], in0=x1, in1=cosb, op=ALU.mult), reads=rk, writes=["ropet0"])
                    P.op("dve", lambda e: e.tensor_tensor(out=t[1], in0=x2, in1=sinb, op=ALU.mult), reads=rk, writes=["ropet1"])
                    P.op("dve", lambda e: e.tensor_tensor(out=t[2], in0=x2, in1=cosb, op=ALU.mult), reads=rk, writes=["ropet2"])
                    P.op("dve", lambda e: e.tensor_tensor(out=t[3], in0=x1, in1=sinb, op=ALU.mult), reads=rk, writes=["ropet3"])
                    dsts = [dst_ap[:, :, 0, :], dst_ap[:, :, 1, :]] if dup else [dst_ap]
                    for d_ in dsts:
                        P.op("dve", lambda e, d_=d_: e.tensor_tensor(out=d_[:, :, 0:half], in0=t[0], in1=t[1], op=ALU.subtract), reads=["ropet0", "ropet1"], writes=[key])
                        P.op("dve", lambda e, d_=d_: e.tensor_tensor(out=d_[:, :, half:2 * half], in0=t[2], in1=t[3], op=ALU.add), reads=["ropet2", "ropet3"], writes=[key])
                        P.op("pool", lambda e, d_=d_: e.tensor_copy(out=d_[:, :, 2 * half:D], in_=src_ap[:, :, 2 * half:D]), reads=["Pf"], writes=[key])

                P.stage(4.05)
                P.op("pool", lambda e: e.tensor_copy(out=Tb[:, TB_AQ:TB_AQ + 320], in_=Pf[:, O_AQ:O_AQ + 320]), reads=["Pf"], writes=["Tb"])
                P.op("pool", lambda e: e.tensor_copy(out=Tb[:, TB_KCV:TB_KCV + 128], in_=Pf[:, O_AKC:O_AKC + 128]), reads=["Pf"], writes=["Tb"])
                P.stage(4.1)
                rope_set(Pf[:, O_AQ:O_AQ + 320].rearrange("p (h d) -> p h d", d=64), 5,
                         Tb[:, TB_AQR:TB_AQR + 640].rearrange("p (h u d) -> p h u d", u=2, d=64), csh, 8, 64, dup=True)
                rope_set(Pf[:, O_AKS:O_AKS + 256].rearrange("p (h d) -> p h d", d=128)[:, :, 0:64], 2,
                         Tb[:, TB_KA:TB_KA + 128].rearrange("p (h d) -> p h d", d=64), csh, 8, 64)
                rope_set(Pf[:, O_BQ:O_BQ + 384].rearrange("p (h d) -> p h d", d=64), 6,
                         Tb[:, TB_B:TB_B + 384].rearrange("p (h d) -> p h d", d=64), csh, 8, 64)
                rope_set(Pf[:, O_CQ:O_CQ + 768].rearrange("p (h d) -> p h d", d=64), 12,
                         Tb[:, TB_C:TB_C + 768].rearrange("p (h d) -> p h d", d=64), csh, 8, 64)
                P.stage(4.2)
                rope_set(Pf[:, O_BIQ:O_BIQ + 288].rearrange("p (h d) -> p h d", d=32), 9, iqf[:], csi, 4, 32, key="iqf")
                P.stage(4.3)
                cidx = 1.0 / math.sqrt(32.0) / math.sqrt(8.0)
                P.op("dve", lambda e: e.tensor_scalar(out=iws[:, 2, :], in0=Pf[:, O_BIW:O_BIW + 8], scalar1=-1.0, scalar2=None, op0=ALU.mult), reads=["Pf"], writes=["iws2"])
                P.op("dve", lambda e: e.tensor_tensor(out=iws[:, 0, :], in0=iws[:, 2, :], in1=Pf[:, O_BIW:O_BIW + 8], op=ALU.max), reads=["Pf", "iws2"], writes=["iws0"])
                P.op("dve", lambda e: e.tensor_scalar(out=iws[:, 0, :], in0=iws[:, 0, :], scalar1=cidx, scalar2=None, op0=ALU.mult), reads=["iws0"], writes=["iws0"])
                P.op("dve", lambda e: e.tensor_scalar(out=iws[:, 2, :], in0=Pf[:, O_BIW:O_BIW + 8], scalar1=0.0, scalar2=2.0, op0=ALU.is_ge, op1=ALU.mult), reads=["Pf"], writes=["iws2"])
                P.op("dve", lambda e: e.tensor_scalar(out=iws[:, 1, :], in0=iws[:, 2, :], scalar1=-1.0, scalar2=None, op0=ALU.add), reads=["iws2"], writes=["iws1"])
                P.op("dve", lambda e: e.tensor_tensor(out=Tb[:, TB_QI:TB_QI + 256].rearrange("p (h d) -> p h d", d=32), in0=iqf[:, 0:8, :],
                                                      in1=iws[:, 0, :].unsqueeze(2).to_broadcast([128, 8, 32]), op=ALU.mult), reads=["iqf", "iws0"], writes=["Tb"])
                P.stage(4.4)
                P.op("pool", lambda e: e.tensor_copy(out=Tb[:, TB_KI:TB_KI + 128].rearrange("p (r d) -> p r d", d=32), in_=iqf[:, 8:9, :].to_broadcast([128, 4, 32])), reads=["iqf"], writes=["Tb"])
                P.stage(4.5)
                for h in range(8):
                    P.op("pool", lambda e, h=h: e.tensor_scalar(out=Dh[:, h, :], in0=ident[:], scalar1=iws[:, 1, h:h + 1], scalar2=None, op0=ALU.mult), reads=["ident", "iws1"], writes=["Dh"])
                P.stage(4.6)
                P.op("act", lambda e: e.activation(out=gs[:], in_=Pf[:, O_AG:O_AG + 15], func=AF.Exp, scale=-1.0), reads=["Pf"], writes=["gs"])
                P.op("dve", lambda e: e.tensor_scalar(out=gs[:], in0=gs[:], scalar1=1.0, scalar2=None, op0=ALU.add), reads=["gs"], writes=["gs"])
                P.op("dve", lambda e: e.reciprocal(out=gs[:], in_=gs[:]), reads=["gs"], writes=["gs"])
                P.stage(4.7)
                P.op("pool", lambda e: e.tensor_copy(out=Vs_a[:, n, 0:64], in_=Pf[:, O_AVS:O_AVS + 64]), reads=["Pf"], writes=["Vs_a:%d" % n])
                P.op("pool", lambda e: e.tensor_copy(out=Vw_a[:, n % 5, 0:64], in_=Pf[:, O_AVW:O_AVW + 64]), reads=["Pf"], writes=["Vw_a:%d" % (n % 5)])
                P.op("pool", lambda e: e.tensor_copy(out=V_b[:, n, 0:64], in_=Pf[:, O_BV:O_BV + 64]), reads=["Pf"], writes=["V_b:%d" % n])
                for g in range(3):
                    sl = n % DIL_R[g]
                    P.op("pool", lambda e, g=g, sl=sl: e.tensor_copy(out=V_c[g][:, sl, :, 0:64], in_=Pf[:, O_CV + g * 128:O_CV + (g + 1) * 128].rearrange("p (h d) -> p h d", d=64)),
                         reads=["Pf"], writes=["V_c%d:%d" % (g, sl)])

                P.stage(5)
                for h in range(5):
                    P.op("pe", lambda e, h=h: e.transpose(out=psT[0:64, h * 128:(h + 1) * 128], in_=Tb[:, TB_AQ + h * 64:TB_AQ + (h + 1) * 64], identity=ident[:]), reads=["Tb", "ident"], writes=["psT"])
                P.op("act", lambda e: e.copy(out=QT_araw[0:64, :], in_=psT[0:64, 0:640]), reads=["psT"], writes=["cmp_Q"])
                P.stage(5.1)
                for h in range(5):
                    P.op("pe", lambda e, h=h: e.transpose(out=psT[:, h * 128:(h + 1) * 128], in_=Tb[:, TB_AQR + h * 128:TB_AQR + (h + 1) * 128], identity=ident[:]), reads=["Tb", "ident"], writes=["psT"])
                P.op("dve", lambda e: e.tensor_copy(out=QT_ar[:], in_=psT[:, 0:640]), reads=["psT"], writes=["slc_Q", "win_Q"])
                P.stage(5.2)
                P.stage(5.2 + 0.01 * 1)
                P.op("pe", lambda e: e.transpose(out=psT[:, 0:128], in_=Tb[:, TB_KA:TB_KA + 128], identity=ident[:]), reads=["Tb", "ident"], writes=["psT"])
                P.stage(5.2 + 0.01 * 2)
                P.op("pe", lambda e: e.transpose(out=psT[:, 128:256], in_=Tb[:, TB_KCV:TB_KCV + 128], identity=ident[:]), reads=["Tb", "ident"], writes=["psT"])
                P.stage(5.2 + 0.01 * 3)
                P.op("pe", lambda e: e.transpose(out=psT[:, 256:384], in_=Tb[:, TB_QI:TB_QI + 128], identity=ident[:]), reads=["Tb", "ident"], writes=["psT"])
                P.stage(5.2 + 0.01 * 4)
                P.op("pe", lambda e: e.transpose(out=psT[:, 384:512], in_=Tb[:, TB_QI + 128:TB_QI + 256], identity=ident[:]), reads=["Tb", "ident"], writes=["psT"])
                P.stage(5.2 + 0.01 * 5)
                P.op("pe", lambda e: e.transpose(out=psT[:, 512:640], in_=Tb[:, TB_KI:TB_KI + 128], identity=ident[:]), reads=["Tb", "ident"], writes=["psT"])
                P.stage(5.2 + 0.01 * 6)
                P.op("act", lambda e: e.copy(out=KT_a[:, T0:T0 + 128], in_=psT[:, 0:128]), reads=["psT"], writes=["KT_a:%d" % n])
                P.stage(5.2 + 0.01 * 7)
                P.op("act", lambda e: e.copy(out=kcvT[:, T0:T0 + 128], in_=psT[:, 128:256]), reads=["psT"], writes=["kcvT:%d" % n])
                P.stage(5.2 + 0.01 * 8)
                P.op("dve", lambda e: e.tensor_copy(out=qiT[:].rearrange("p a t -> p (a t)"), in_=psT[:, 256:512]), reads=["psT"], writes=["qiT"])
                P.stage(5.2 + 0.01 * 9)
                P.op("dve", lambda e: e.tensor_copy(out=kiT4[:, T0:T0 + 128], in_=psT[:, 512:640]), reads=["psT"], writes=["kiT4:%d" % n])
                P.stage(5.3)
                for h in range(6):
                    P.op("pe", lambda e, h=h: e.transpose(out=psT[0:64, h * 128:(h + 1) * 128], in_=Tb[:, TB_B + h * 64:TB_B + (h + 1) * 64], identity=ident[:]), reads=["Tb", "ident"], writes=["psT"])
                P.op("act", lambda e: e.copy(out=QT_b[0:64, :], in_=psT[0:64, 0:640]), reads=["psT"], writes=["dsa_Q"])
                P.op("dve", lambda e: e.tensor_copy(out=KT_b[0:64, T0:T0 + 128], in_=psT[0:64, 640:768]), reads=["psT"], writes=["KT_b:%d" % n])
                P.stage(5.4)
                for k in range(6):
                    P.op("pe", lambda e, k=k: e.transpose(out=psT[:, k * 128:(k + 1) * 128], in_=Tb[:, TB_C + k * 128:TB_C + (k + 1) * 128], identity=ident[:]), reads=["Tb", "ident"], writes=["psT"])
                P.op("act", lambda e: e.copy(out=QT_c[:].rearrange("p a t -> p (a t)"), in_=psT[:, 0:384]), reads=["psT"], writes=["dil0_Q", "dil1_Q", "dil2_Q"])
                for g in range(3):
                    sl = n % DIL_R[g]
                    P.op("dve", lambda e, g=g, sl=sl: e.tensor_copy(out=KT_c[g][:, sl, :], in_=psT[:, 384 + g * 128:384 + (g + 1) * 128]), reads=["psT"], writes=["KT_c%d:%d" % (g, sl)])

                P.stage(6)
                j0 = 0 if n == 0 else 8 * n - 1
                nb = 7 if n == 0 else 8
                if n == NT - 1:
                    nb = 8
                tk0 = 16 * j0
                rk_kcv = ["kcvT:%d" % n] + (["kcvT:%d" % (n - 1)] if n > 0 else [])
                for c in range(2):
                    for jj in range(32):
                        a0 = tk0 + jj
                        P.op("pe", lambda e, c=c, jj=jj, a0=a0: e.matmul((psP if c == 0 else psA)[:, 0:nb], lhsT=W1_sb[c * 64:(c + 1) * 64, jj, :],
                                                                          rhs=kcvT[c * 64:(c + 1) * 64, a0:a0 + 16 * (nb - 1) + 1:16],
                                                                          start=(jj == 0), stop=(jj == 31), skip_group_check=True),
                             reads=["W1_sb"] + rk_kcv, writes=["psP" if c == 0 else "psA"])
                for c in range(2):
                    P.op("dve", lambda e, c=c: e.tensor_scalar(out=zs[:, 0, c * 16:c * 16 + nb], in0=(psP if c == 0 else psA)[:, 0:nb], scalar1=constc[:, c:c + 1], scalar2=None, op0=ALU.add),
                         reads=["psP" if c == 0 else "psA", "constc"], writes=["zs0"])
                if nb < 16:
                    pass
                zsl = lambda k: zs[:, k, :].rearrange("p (c b) -> p c b", c=2)[:, :, 0:nb]
                P.op("dve", lambda e: e.tensor_tensor(out=zsl(1), in0=zsl(0), in1=zsl(0), op=ALU.mult), reads=["zs0"], writes=["zs1"])
                P.op("dve", lambda e: e.tensor_scalar(out=zsl(1), in0=zsl(1), scalar1=0.044715, scalar2=1.0, op0=ALU.mult, op1=ALU.add), reads=["zs1"], writes=["zs1"])
                P.op("dve", lambda e: e.tensor_tensor(out=zsl(2), in0=zsl(1), in1=zsl(0), op=ALU.mult), reads=["zs0", "zs1"], writes=["zs2"])
                P.op("act", lambda e: e.activation(out=zsl(3), in_=zsl(2), func=AF.Tanh, scale=0.7978845608028654), reads=["zs2"], writes=["zs3"])
                P.op("dve", lambda e: e.tensor_scalar(out=zsl(3), in0=zsl(3), scalar1=1.0, scalar2=0.5, op0=ALU.add, op1=ALU.mult), reads=["zs3"], writes=["zs3"])
                P.op("dve", lambda e: e.tensor_tensor(out=hidT[:].rearrange("p (c b) -> p c b", c=2)[:, :, 0:nb], in0=zsl(3), in1=zsl(0), op=ALU.mult), reads=["zs3", "zs0"], writes=["hidT"])
                P.op("pe", lambda e: e.matmul(psP[0:64, 32:32 + nb], lhsT=W2_sb[:, 0, :], rhs=hidT[:, 0:nb], start=True, stop=True, skip_group_check=True), reads=["W2_sb", "hidT"], writes=["psP"])
                P.op("pe", lambda e: e.matmul(psP[0:nb, 64:128], lhsT=hidT[:, 16:16 + nb], rhs=W2_sb[:, 1, :], start=True, stop=True, skip_group_check=True), reads=["W2_sb", "hidT"], writes=["psP"])
                P.op("dve", lambda e: e.tensor_copy(out=kcmpT[:, j0:j0 + nb], in_=psP[0:64, 32:32 + nb]), reads=["psP"], writes=["kcmpT"])
                P.op("dve", lambda e: e.tensor_copy(out=vst[0:nb, :], in_=psP[0:nb, 64:128]), reads=["psP"], writes=["vst"])
                b = 0
                while b < nb:
                    j = j0 + b
                    tl, pp = j // 128, j % 128
                    cnt = min(nb - b, 128 - pp)
                    P.dma("sp", lambda e, b=b, tl=tl, pp=pp, cnt=cnt: e.dma_start(out=Vc[pp:pp + cnt, tl, 0:64], in_=vst[b:b + cnt, :]), reads=["vst"], writes=["Vc"])
                    b += cnt

                S = (n + 1) * 128
                P.stage(7)
                nch = (S + 511) // 512
                for ch in range(nch):
                    k0 = ch * 512
                    wc = min(512, S - k0)
                    rkk = ["kiT4:%d" % kt for kt in range(k0 // 128, (k0 + wc) // 128)]
                    for half in range(2):
                        for j in range(4):
                            dst = (psA if j < 2 else psB)[:, (j % 2) * 512:(j % 2) * 512 + wc]
                            dk = "psA" if j < 2 else "psB"
                            P.op("pe", lambda e, j=j, half=half, dst=dst, k0=k0, wc=wc: e.matmul(dst, lhsT=qiT[j * 32:(j + 1) * 32, half, :], rhs=kiT4[j * 32:(j + 1) * 32, k0:k0 + wc],
                                                                                                  start=True, stop=True, tile_position=(j * 32, 0), skip_group_check=True),
                                 reads=["qiT"] + rkk, writes=[dk])
                        for j in range(4):
                            src = (psA if j < 2 else psB)[:, (j % 2) * 512:(j % 2) * 512 + wc]
                            dk = "psA" if j < 2 else "psB"
                            if j % 2 == 0:
                                P.op("act", lambda e, j=j, src=src, wc=wc: e.activation(out=Rbuf[:, j, 0:wc], in_=src, func=AF.Relu), reads=[dk], writes=["R%d" % j])
                            else:
                                P.op("dve", lambda e, j=j, src=src, wc=wc: e.tensor_scalar(out=Rbuf[:, j, 0:wc], in0=src, scalar1=0.0, scalar2=None, op0=ALU.max), reads=[dk], writes=["R%d" % j])
                        for j in range(4):
                            h = half * 4 + j
                            P.op("pe", lambda e, h=h, j=j, wc=wc, ch=ch: e.matmul(psP[:, 0:wc], lhsT=Dh[:, h, :], rhs=Rbuf[:, j, 0:wc], start=(h == 0), stop=(h == 7 and ch != nch - 1), skip_group_check=True),
                                 reads=["Dh", "R%d" % j], writes=["psP"])
                    if ch == nch - 1:
                        P.op("pe", lambda e, wc=wc: e.matmul(psP[:, wc - 128:wc], lhsT=ident[:], rhs=biasT[:, NB_BIAS - 1, :], start=False, stop=True, skip_group_check=True),
                             reads=["ident", "biasT"], writes=["psP"])
                    P.op("act", lambda e, k0=k0, wc=wc: e.copy(out=score[:, k0:k0 + wc], in_=psP[:, 0:wc]), reads=["psP"], writes=["score"])
                P.stage(8)
                P.op("pool", lambda e: e.memset(bis[:, 0:1], BIS_LO), writes=["bis_lo"])
                if n >= 2:
                    w = -BIS_LO
                    P.op("pool", lambda e: e.memset(bis[:, 1:2], 0.0), writes=["bis_mid"])
                    for it in range(BIS_ITERS):
                        P.op("dve", lambda e: e.tensor_scalar(out=mb_d[:, 0:S], in0=score[:, 0:S], scalar1=bis[:, 1:2], scalar2=None, op0=ALU.is_ge, op1=ALU.add, accum_out=bis[:, 2:3]),
                             reads=["score", "bis_mid"], writes=["mb", "bis_cnt"])
                        P.op("dve", lambda e, w=w: e.tensor_scalar(out=bis[:, 3:4], in0=bis[:, 2:3], scalar1=255.5, scalar2=w, op0=ALU.is_ge, op1=ALU.mult), reads=["bis_cnt"], writes=["bis_g"])
                        P.op("dve", lambda e: e.tensor_tensor(out=bis[:, 0:1], in0=bis[:, 0:1], in1=bis[:, 3:4], op=ALU.add), reads=["bis_lo", "bis_g"], writes=["bis_lo"])
                        w = w / 2.0
                        P.op("dve", lambda e, w=w: e.tensor_scalar(out=bis[:, 1:2], in0=bis[:, 0:1], scalar1=w, scalar2=None, op0=ALU.add), reads=["bis_lo"], writes=["bis_mid"])
                P.op("dve", lambda e: e.tensor_scalar(out=mb_d[:, 0:S], in0=score[:, 0:S], scalar1=bis[:, 0:1], scalar2=MASKV, op0=ALU.is_lt, op1=ALU.mult), reads=["score", "bis_lo"], writes=["mb"])
                items = []
                for kt in range(max(0, n - 4), n + 1):
                    bias_ap = biasT[:, 0, :] if kt == n else (biasT[:, 1, :] if kt == n - 4 else None)
                    items.append((lambda h, kt=kt: KT_a[64:128, kt * 128:(kt + 1) * 128], lambda h, kt=kt: Vw_a[:, kt % 5, :], bias_ap,
                                  ["KT_a:%d" % kt, "Vw_a:%d" % (kt % 5), "Vw_a_ones", "biasT"], True))
                attention("win", 5, 65, items, lambda h0, h1: QT_ar[64:128, h0 * 128:h1 * 128], lambda h: (psO[:, 512 + h * 65:512 + (h + 1) * 65], "psO1"), None, None, None)
                P.op("act", lambda e: e.copy(out=Owin[:].rearrange("p h w -> p (h w)"), in_=psO[:, 512:512 + 325]), reads=["psO1"], writes=["Owin"])

                P.stage(12)
                for g in range(3):
                    items = []
                    for o in range(DIL_O[g] + 1):
                        kt = n - o
                        if kt < 0:
                            continue
                        sl = kt % DIL_R[g]
                        items.append((lambda h, g=g, sl=sl: KT_c[g][h * 64:(h + 1) * 64, sl, :], lambda h, g=g, sl=sl: V_c[g][:, sl, h, :], biasT[:, _dil_bias_idx(g, o), :],
                                      ["KT_c%d:%d" % (g, sl), "V_c%d:%d" % (g, sl), "V_c_ones%d" % g, "biasT"], False))
                    bk = (g + 1) % 2
                    attention("dil%d" % g, 2, 65, items, lambda h0, h1, g=g: QT_c[h0 * 64:(h0 + 1) * 64, g, :],
                              lambda h, bk=bk: (psO[:, bk * 512 + h * 65:bk * 512 + (h + 1) * 65], "psO%d" % bk), None, None, None)
                    P.op("act", lambda e, g=g, bk=bk: e.copy(out=Odil[:, g, :, :].rearrange("p h w -> p (h w)"), in_=psO[:, bk * 512:bk * 512 + 130]), reads=["psO%d" % bk], writes=["Odil%d" % g])

                P.stage(9)
                items = [(lambda h, kt=kt: KT_b[0:64, kt * 128:(kt + 1) * 128], lambda h, kt=kt: V_b[:, kt, :], mb_d[:, kt * 128:(kt + 1) * 128],
                          ["KT_b:%d" % kt, "V_b:%d" % kt, "V_b_ones", "mb"], True) for kt in range(n + 1)]
                attention("dsa", 5, 65, items, lambda h0, h1: QT_b[0:64, h0 * 128:h1 * 128], lambda h: (psO[:, h * 65:(h + 1) * 65], "psO0"), None, None, None)
                P.op("act", lambda e: e.copy(out=Odsa[:].rearrange("p h w -> p (h w)"), in_=psO[:, 0:325]), reads=["psO0"], writes=["Odsa", "score"])
                P.stage(10)
                items = []
                for nt_ in range(2):
                    base = 128 * n - 2048 * nt_ - 31
                    if base + 127 < 0:
                        continue
                    bias_ap = None
                    if base < 16 * 127:
                        cbt = mb_s[:, nt_ * 128:(nt_ + 1) * 128]
                        ci = cmp_pairs.index((n, nt_))
                        P.dma("pool", lambda e, cbt=cbt, ci=ci: e.dma_start(out=cbt, in_=c_cmpb[ci]), writes=["mb"])
                        bias_ap = cbt
                    items.append((lambda h, nt_=nt_: kcmpT[:, nt_ * 128:(nt_ + 1) * 128], lambda h, nt_=nt_: Vc[:, nt_, :], bias_ap, ["kcmpT", "Vc", "mb"], True))

                def ops_cmp(h):
                    bk = h // 3
                    return psO[:, bk * 512 + (h % 3) * 129: bk * 512 + (h % 3 + 1) * 129], "psO%d" % bk
                P.stage(10.1)
                attention("cmp", 5, 129, items, lambda h0, h1: QT_araw[0:64, h0 * 128:h1 * 128], ops_cmp, None, None, None)
                P.stage(10.2)
                P.op("act", lambda e: e.copy(out=Ocmp[:, 0:3, :].rearrange("p h w -> p (h w)"), in_=psO[:, 0:387]), reads=["psO0"], writes=["Ocmp", "score"])
                P.op("act", lambda e: e.copy(out=Ocmp[:, 3:5, :].rearrange("p h w -> p (h w)"), in_=psO[:, 512:512 + 258]), reads=["psO1"], writes=["Ocmp", "score"])
                P.stage(10.3)
                P.op("dve", lambda e: e.tensor_scalar(out=rdb[:, 0:5], in0=Ocmp[:, :, 64], scalar1=1e-30, scalar2=None, op0=ALU.max), reads=["Ocmp"], writes=["rdb"])
                P.op("dve", lambda e: e.reciprocal(out=rdb[:, 0:5], in_=rdb[:, 0:5]), reads=["rdb"], writes=["rdb"])
                P.stage(10.4)
                P.op("pool", lambda e: e.memset(fb[:], 0.0), writes=["fb"])
                if 2 * n + 1 < 64:
                    P.op("pool", lambda e: e.memset(fb[0:64, 2 * n + 1:64], NEGBIG), writes=["fb"])
                if 2 * n + 2 < 64:
                    P.op("pool", lambda e: e.memset(fb[64:128, 2 * n + 2:64], NEGBIG), writes=["fb"])
                P.op("pool", lambda e: e.memset(fb[:, 0:1], 1e9), writes=["fb"])
                P.op("pool", lambda e: e.memset(fb[0:64, 2 * n:2 * n + 1], 1e9), writes=["fb"])
                if n >= 1:
                    P.op("pool", lambda e: e.memset(fb[0:64, 2 * n - 1:2 * n], 1e9), writes=["fb"])
                P.op("pool", lambda e: e.memset(fb[64:128, 2 * n:2 * n + 2], 1e9), writes=["fb"])
                P.stage(10.5)
                for h in range(5):
                    P.op("dve", lambda e, h=h: e.scalar_tensor_tensor(out=impf[:, 0, :], in0=Ocmp[:, h, 65:129], scalar=rdb[:, h:h + 1], in1=(fb[:] if h == 0 else impf[:, 0, :]), op0=ALU.mult, op1=ALU.add),
                         reads=["Ocmp", "rdb", "fb", "impf0"], writes=["impf0"])
                P.stage(10.6)
                P.op("dve", lambda e: e.max(out=m8[:, 0:8], in_=impf[:, 0, :]), reads=["impf0"], writes=["m8a"])
                P.op("dve", lambda e: e.match_replace(out=impf[:, 1, :], in_to_replace=m8[:, 0:8], in_values=impf[:, 0, :], imm_value=-3.0e38), reads=["m8a", "impf0"], writes=["impf1"])
                P.op("dve", lambda e: e.max(out=m8[:, 8:16], in_=impf[:, 1, :]), reads=["impf1"], writes=["m8b"])
                P.op("dve", lambda e: e.tensor_scalar(out=m8[:, 0:1], in0=m8[:, 15:16], scalar1=-1e29, scalar2=None, op0=ALU.max), reads=["m8b", "m8a"], writes=["m8a"])
                P.op("dve", lambda e: e.tensor_scalar(out=bmb[:], in0=impf[:, 0, :], scalar1=m8[:, 0:1], scalar2=MASKV, op0=ALU.is_lt, op1=ALU.mult), reads=["impf0", "m8a"], writes=["bmb"])
                P.stage(10.7)
                S = (n + 1) * 128
                nbk = S // 64
                P.op("dve", lambda e: e.tensor_copy(out=mb_s[:, 0:S].rearrange("p (j k) -> p j k", k=64), in_=bmb[:, 0:nbk].unsqueeze(2).to_broadcast([128, nbk, 64])), reads=["bmb"], writes=["mb"])
                P.op("dve", lambda e: e.tensor_tensor(out=mb_s[:, T0:T0 + 128], in0=mb_s[:, T0:T0 + 128], in1=biasT[:, 0, :], op=ALU.min), reads=["mb", "biasT"], writes=["mb"])

                P.stage(11)
                items = [(lambda h, kt=kt: KT_a[0:64, kt * 128:(kt + 1) * 128], lambda h, kt=kt: Vs_a[:, kt, :], mb_s[:, kt * 128:(kt + 1) * 128],
                          ["KT_a:%d" % kt, "Vs_a:%d" % kt, "Vs_a_ones", "mb"], True) for kt in range(n + 1)]
                attention("slc", 5, 65, items, lambda h0, h1: QT_ar[0:64, h0 * 128:h1 * 128], lambda h: (psO[:, h * 65:(h + 1) * 65], "psO0"), None, None, None)
                P.op("act", lambda e: e.copy(out=Oslc[:].rearrange("p h w -> p (h w)"), in_=psO[:, 0:325]), reads=["psO0"], writes=["Oslc", "score"])
                P.stage(13)
                dv = dens[:].rearrange("p (h b) -> p h b", b=3)
                P.op("dve", lambda e: e.tensor_copy(out=dv[:, :, 0], in_=Ocmp[:, :, 64]), reads=["Ocmp"], writes=["dens"])
                P.op("dve", lambda e: e.tensor_copy(out=dv[:, :, 1], in_=Oslc[:, :, 64]), reads=["Oslc"], writes=["dens"])
                P.op("dve", lambda e: e.tensor_copy(out=dv[:, :, 2], in_=Owin[:, :, 64]), reads=["Owin"], writes=["dens"])
                P.op("dve", lambda e: e.tensor_scalar(out=dens[:], in0=dens[:], scalar1=1e-30, scalar2=None, op0=ALU.max), reads=["dens"], writes=["dens"])
                P.op("dve", lambda e: e.reciprocal(out=dens[:], in_=dens[:]), reads=["dens"], writes=["dens"])
                P.op("dve", lambda e: e.tensor_tensor(out=coef[:], in0=dens[:], in1=gs[:], op=ALU.mult), reads=["dens", "gs"], writes=["coef"])
                cv = coef[:].rearrange("p (h b) -> p h b", b=3)
                P.op("dve", lambda e: e.tensor_tensor(out=tmpo[:, 0], in0=Ocmp[:, :, 0:64], in1=cv[:, :, 0:1].to_broadcast([128, 5, 64]), op=ALU.mult), reads=["Ocmp", "coef"], writes=["tmpo0"])
                P.op("dve", lambda e: e.tensor_tensor(out=tmpo[:, 1], in0=Oslc[:, :, 0:64], in1=cv[:, :, 1:2].to_broadcast([128, 5, 64]), op=ALU.mult), reads=["Oslc", "coef"], writes=["tmpo1"])
                P.op("dve", lambda e: e.tensor_tensor(out=tmpo[:, 0], in0=tmpo[:, 0], in1=tmpo[:, 1], op=ALU.add), reads=["tmpo0", "tmpo1"], writes=["tmpo0"])
                P.op("dve", lambda e: e.tensor_tensor(out=tmpo[:, 1], in0=Owin[:, :, 0:64], in1=cv[:, :, 2:3].to_broadcast([128, 5, 64]), op=ALU.mult), reads=["Owin", "coef"], writes=["tmpo1"])
                P.op("dve", lambda e: e.tensor_tensor(out=mixb[:, 0:320].rearrange("p (h d) -> p h d", d=64), in0=tmpo[:, 0], in1=tmpo[:, 1], op=ALU.add), reads=["tmpo0", "tmpo1"], writes=["mixb"])
                P.op("dve", lambda e: e.tensor_scalar(out=rdb[:, 8:13], in0=Odsa[:, :, 64], scalar1=1e-30, scalar2=None, op0=ALU.max), reads=["Odsa"], writes=["rdb2"])
                P.op("dve", lambda e: e.reciprocal(out=rdb[:, 8:13], in_=rdb[:, 8:13]), reads=["rdb2"], writes=["rdb2"])
                P.op("dve", lambda e: e.tensor_tensor(out=mixb[:, 320:640].rearrange("p (h d) -> p h d", d=64), in0=Odsa[:, :, 0:64], in1=rdb[:, 8:13].unsqueeze(2).to_broadcast([128, 5, 64]), op=ALU.mult),
                     reads=["Odsa", "rdb2"], writes=["mixb"])
                P.op("dve", lambda e: e.tensor_tensor(out=rdb[:, 13:15], in0=Odil[:, 0, :, 64], in1=Odil[:, 1, :, 64], op=ALU.add), reads=["Odil0", "Odil1"], writes=["rdb3"])
                P.op("dve", lambda e: e.tensor_tensor(out=rdb[:, 13:15], in0=rdb[:, 13:15], in1=Odil[:, 2, :, 64], op=ALU.add), reads=["rdb3", "Odil2"], writes=["rdb3"])
                P.op("dve", lambda e: e.reciprocal(out=rdb[:, 13:15], in_=rdb[:, 13:15]), reads=["rdb3"], writes=["rdb3"])
                for g in range(3):
                    P.op("dve", lambda e, g=g: e.tensor_tensor(out=mixb[:, 640 + g * 128:640 + (g + 1) * 128].rearrange("p (h d) -> p h d", d=64), in0=Odil[:, g, :, 0:64],
                                                               in1=rdb[:, 13:15].unsqueeze(2).to_broadcast([128, 2, 64]), op=ALU.mult), reads=["Odil%d" % g, "rdb3"], writes=["mixb"])
                if dbg and li == 0:
                    P.op("dve", lambda e: e.tensor_copy(out=score[:, 0:DM], in_=mixb[:]), reads=["mixb"], writes=["score"])
                    P.dma("sp", lambda e: e.dma_start(out=dbg_out["d_mix"][T0:T0 + 128, :], in_=score[:, 0:DM]), reads=["score"], writes=["d_mix"])
                for c in range(8):
                    P.op("pe", lambda e, c=c: e.transpose(out=psT[:, c * 128:(c + 1) * 128], in_=mixb[:, c * 128:(c + 1) * 128], identity=ident[:]), reads=["mixb", "ident"], writes=["psT"])
                P.op("act", lambda e: e.copy(out=mixT[:].rearrange("p c t -> p (c t)"), in_=psT[:, :]), reads=["psT"], writes=["hT"])
                for hf in range(2):
                    for c in range(8):
                        P.op("pe", lambda e, c=c, hf=hf: e.matmul(psA[:, hf * 512:(hf + 1) * 512], lhsT=mixT[:, c, :], rhs=w_out_sb[:, c, hf * 512:(hf + 1) * 512], start=(c == 0), stop=(c == 7)),
                             reads=["hT", "w_out_sb"], writes=["psA"])
                P.op("dve", lambda e, xb_=xb_: e.tensor_tensor(out=x1t[:], in0=psA[:, :], in1=xb_[:], op=ALU.add), reads=["psA", xk], writes=["xt"])
                P.dma("sp", lambda e: e.dma_start(out=xs1[T0:T0 + 128, :], in_=x1t[:]), reads=["xt"], writes=["xs1:%d" % n])
                if dbg and li == 0:
                    P.dma("sp", lambda e: e.dma_start(out=dbg_out["d_x1"][T0:T0 + 128, :], in_=x1t[:]), reads=["xt"], writes=["d_x1"])

            for n in range(NTT):
                tile_body(n)

            P.stage(14)
            P.barrier()
            for c in range(8):
                P.dma("pool", lambda e, c=c: e.dma_start(out=w_up_sb[:, c, :], in_=w_up[li, c * 128:(c + 1) * 128, :]), writes=["w_up_sb"])
            for c in range(NFC):
                P.dma("pool", lambda e, c=c: e.dma_start(out=w_dn_sb[:, c, :], in_=w_down[li, c * 128:(c + 1) * 128, :]), writes=["w_dn_sb"])
            P.dma("sp", lambda e: e.dma_start(out=cwst[:, 0:3, :], in_=w_cw[li].rearrange("k (c p) -> c k p", p=128)), writes=["cwst"])
            P.dma("sp", lambda e: e.dma_start(out=cwst[:, 3, :], in_=w_cb[li].rearrange("(c p) -> c p", p=128)), writes=["cwst"])
            for k in range(4):
                P.op("pe", lambda e, k=k: e.transpose(out=psP[:, k * 32:k * 32 + NFC], in_=cwst[:, k, :], identity=identf[0:NFC, 0:NFC]), reads=["cwst", "identf"], writes=["psP"])
            P.op("dve", lambda e: e.tensor_copy(out=cwall[:], in_=psP[:, 0:128].rearrange("p (k c) -> p k c", k=4)[:, :, 0:NFC]), reads=["psP"], writes=["cwall"])
            P.op("pool", lambda e: e.memset(carry[:], 0.0), writes=["carry"])
            if is_last and final:
                P.dma("sp", lambda e: e.dma_start(out=gfbc, in_=w_fin.partition_broadcast(128)), writes=["gfbc"])
            P.stage(15)
            NG = (NTT * 128 + TF - 1) // TF
            def ffn_group(gi):
                G0 = gi * TF
                nsub = min(TF // 128, NTT - gi * (TF // 128))
                TW = nsub * 128
                for sb_ in range(nsub):
                    P.dma("sp", lambda e, sb_=sb_: e.dma_start(out=xf[:, sb_, :], in_=xs1[G0 + sb_ * 128:G0 + (sb_ + 1) * 128, :]), reads=["xs1:%d" % (gi * (TF // 128) + sb_)], writes=["xf%d" % sb_])
                    P.op("dve", lambda e, sb_=sb_: e.scalar_tensor_tensor(out=junkb[:], in0=xf[:, sb_, :], scalar=1.0, in1=xf[:, sb_, :], op0=ALU.mult, op1=ALU.mult, accum_out=ss[:, 0:1]),
                         reads=["xf%d" % sb_], writes=["hb", "ss0"])
                    P.op("dve", lambda e: e.tensor_scalar(out=ss[:, 1:2], in0=ss[:, 0:1], scalar1=1.0 / DM, scalar2=EPS, op0=ALU.mult, op1=ALU.add), reads=["ss0"], writes=["ss1"])
                    P.op("pool!", lambda e: e.tensor_tensor(out=ss[:, 2:3], in0=ss[:, 1:2], in1=neghalf[:], op=ALU.pow), reads=["ss1", "neghalf"], writes=["ss2"])
                    P.op("dve", lambda e, sb_=sb_: e.tensor_scalar(out=hb[:], in0=xf[:, sb_, :], scalar1=ss[:, 2:3], scalar2=None, op0=ALU.mult),
                         reads=["xf%d" % sb_, "ss2"], writes=["hb"])
                    for c in range(8):
                        P.op("pe", lambda e, c=c: e.transpose(out=psT[:, c * 128:(c + 1) * 128], in_=hb[:, c * 128:(c + 1) * 128], identity=ident[:]), reads=["hb", "ident"], writes=["psT"])
                    P.op("dve", lambda e, sb_=sb_: e.tensor_tensor(out=h2T[:, :, sb_ * 128:(sb_ + 1) * 128], in0=psT[:, :].rearrange("p (c t) -> p c t", c=8),
                                                                 in1=gcol[:, 8:16].unsqueeze(2).to_broadcast([128, 8, 128]), op=ALU.mult), reads=["psT", "gcol"], writes=["h2T"])
                for fc in range(NFC):
                    pa = psA[:, 0:TW]
                    pu = psA[:, 512:512 + TW]
                    if fc % 2 == 1:
                        pa = psB[:, 0:TW]
                        pu = psB[:, 512:512 + TW]
                    pk = "psA" if fc % 2 == 0 else "psB"
                    for c in range(8):
                        P.op("pe", lambda e, c=c, fc=fc, pa=pa: e.matmul(pa, lhsT=w_up_sb[:, c, fc * 128:(fc + 1) * 128], rhs=h2T[:, c, 0:TW], start=(c == 0), stop=(c == 7)),
                             reads=["w_up_sb", "h2T"], writes=[pk])
                    for c in range(8):
                        P.op("pe", lambda e, c=c, fc=fc, pu=pu: e.matmul(pu, lhsT=w_up_sb[:, c, DFF + fc * 128:DFF + (fc + 1) * 128], rhs=h2T[:, c, 0:TW], start=(c == 0), stop=(c == 7)),
                             reads=["w_up_sb", "h2T"], writes=[pk])
                    P.op("pool", lambda e, fc=fc: e.tensor_copy(out=abuf[:, 0:2], in_=carry[:, fc, :]), reads=["carry"], writes=["abuf"])
                    P.op("act", lambda e, pa=pa: e.copy(out=abuf[:, 2:TW + 2], in_=pa), reads=[pk], writes=["abuf"])
                    P.op("pool", lambda e, fc=fc: e.tensor_copy(out=carry[:, fc, :], in_=abuf[:, TW:TW + 2]), reads=["abuf"], writes=["carry"])
                    P.op("dve", lambda e, fc=fc: e.tensor_scalar(out=accb[:, 0:TW], in0=abuf[:, 2:TW + 2], scalar1=cwall[:, 2, fc:fc + 1], scalar2=cwall[:, 3, fc:fc + 1], op0=ALU.mult, op1=ALU.add),
                         reads=["abuf", "cwall"], writes=["accb"])
                    P.op("dve", lambda e, fc=fc: e.scalar_tensor_tensor(out=accb[:, 0:TW], in0=abuf[:, 1:TW + 1], scalar=cwall[:, 1, fc:fc + 1], in1=accb[:, 0:TW], op0=ALU.mult, op1=ALU.add),
                         reads=["abuf", "cwall", "accb"], writes=["accb"])
                    P.op("dve", lambda e, fc=fc: e.scalar_tensor_tensor(out=accb[:, 0:TW], in0=abuf[:, 0:TW], scalar=cwall[:, 0, fc:fc + 1], in1=accb[:, 0:TW], op0=ALU.mult, op1=ALU.add),
                         reads=["abuf", "cwall", "accb"], writes=["accb"])
                    P.op("act", lambda e: e.activation(out=silb[:, 0:TW], in_=accb[:, 0:TW], func=AF.Silu), reads=["accb"], writes=["silb"])
                    P.op("dve", lambda e, fc=fc, pu=pu: e.tensor_tensor(out=gT[:, fc, 0:TW], in0=pu, in1=silb[:, 0:TW], op=ALU.mult), reads=[pk, "silb"], writes=["gT"])
                for sb_ in range(nsub):
                    for hf in range(2):
                        for fc in range(NFC):
                            P.op("pe", lambda e, fc=fc, hf=hf, sb_=sb_: e.matmul(psO[:, hf * 512:(hf + 1) * 512], lhsT=gT[:, fc, sb_ * 128:(sb_ + 1) * 128], rhs=w_dn_sb[:, fc, hf * 512:(hf + 1) * 512],
                                                                                 start=(fc == 0), stop=(fc == NFC - 1)), reads=["gT", "w_dn_sb"], writes=["psO%d" % hf])
                    P.op("dve", lambda e, sb_=sb_: e.tensor_tensor(out=x2t[:], in0=psO[:, :], in1=xf[:, sb_, :], op=ALU.add), reads=["psO0", "psO1", "xf%d" % sb_], writes=["x2t"])
                    r0 = G0 + sb_ * 128
                    if is_last and final:
                        P.op("dve", lambda e: e.scalar_tensor_tensor(out=junkb[:], in0=x2t[:], scalar=1.0, in1=x2t[:], op0=ALU.mult, op1=ALU.mult, accum_out=ss[:, 0:1]), reads=["x2t"], writes=["hb", "ss0"])
                        P.op("dve", lambda e: e.tensor_scalar(out=ss[:, 1:2], in0=ss[:, 0:1], scalar1=1.0 / DM, scalar2=EPS, op0=ALU.mult, op1=ALU.add), reads=["ss0"], writes=["ss1"])
                        P.op("pool!", lambda e: e.tensor_tensor(out=ss[:, 2:3], in0=ss[:, 1:2], in1=neghalf[:], op=ALU.pow), reads=["ss1", "neghalf"], writes=["ss2"])
                        P.op("dve", lambda e: e.scalar_tensor_tensor(out=x2t[:], in0=x2t[:], scalar=ss[:, 2:3], in1=gfbc[:], op0=ALU.mult, op1=ALU.mult), reads=["x2t", "ss2", "gfbc"], writes=["x2t"])
                    P.dma("sp", lambda e, r0=r0: e.dma_start(out=xout_ap[r0:r0 + 128, :], in_=x2t[:]), reads=["x2t"], writes=["xout:%d" % (r0 // 128)])

            for gi in range(NG):
                ffn_group(gi)

        cur_in = x_in
        for li in range(n_layers):
            is_last = li == n_layers - 1
            xout = y_out if is_last else xs2
            emit_layer(li, cur_in, xout, is_last)
            cur_in = xs2
        P.final_wait("sp", ["xout:%d" % i for i in range(NTT)] + (["d_pf", "d_mix", "d_x1"] if dbg else []))
        P.enabled = True
        P.barrier()
        print("ops recorded:", P.nops, "sbuf remaining:", nc.sbuf_bytes_remaining, "att_end", att_end, "ffn_end", ffn_end)
        P.emit(st)
    return nc


_CACHE = {}


def _get_prog(n_layers, final):
    key = (n_layers, final)
    if key not in _CACHE:
        _CACHE[key] = build(n_layers, final)
    return _CACHE[key]


FUSED = True


def kernel(x, norm1_g, w_in, cmp_pos, cmp_w1, cmp_w2, w_out, norm2_g, w_up, conv_w, conv_b, w_down, final_g):
    f = lambda a: np.ascontiguousarray(np.asarray(a, dtype=np.float32))
    x = f(x)
    W = dict(norm1_g=f(norm1_g), w_in=f(w_in), cmp_pos=f(cmp_pos), cmp_w1=f(cmp_w1), cmp_w2=f(cmp_w2), w_out=f(w_out),
             norm2_g=f(norm2_g), w_up=f(w_up), conv_w=f(conv_w), conv_b=f(conv_b), w_down=f(w_down))
    consts = _consts()
    B = x.shape[0]
    depth = W["w_in"].shape[0]
    cur = x
    if FUSED:
        nc = _get_prog(depth, True)
        in_maps = []
        for c in range(8):
            m = dict(x=cur[c % B], final_g=f(final_g))
            m.update(W)
            m.update(consts)
            in_maps.append(m)
        res = run_bass_kernel_spmd(nc, in_maps, core_ids=list(range(8)))
        return np.stack([res.results[b]["y"] for b in range(B)], axis=0)
    for li in range(depth):
        last = li == depth - 1
        nc = _get_prog(1, last)
        in_maps = []
        for c in range(8):
            m = dict(x=np.ascontiguousarray(cur[c % B]), final_g=f(final_g))
            m.update({k: np.ascontiguousarray(v[li:li + 1]) for k, v in W.items()})
            m.update(consts)
            in_maps.append(m)
        res = run_bass_kernel_spmd(nc, in_maps, core_ids=list(range(8)))
        cur = np.stack([res.results[b]["y"] for b in range(B)], axis=0)
    return cur
```
